# Optimizing a Trainium2 kernel written in Bass

```python
import jax
import jax.numpy as jnp
from jax import lax
import numpy as np

D_MODEL = 1024
BATCH = 16
SEQ = 4096
DEPTH = 4

CTX_LEN = 256
GRID_W = 64

POOL_GROUPS = 4
POOL_GROUP_DIM = D_MODEL // 16
POOL_WIDTH = POOL_GROUPS * POOL_GROUP_DIM
POOL_WINDOWS = (2, 4, 8, 16)

MLSTM_HEADS = 4
MLSTM_HEAD_DIM = D_MODEL // 8
MLSTM_WIDTH = MLSTM_HEADS * MLSTM_HEAD_DIM
MLSTM_CHUNK = 128
QK_CONV_WIDTH = 3

SGU_GROUPS = 4
SGU_GROUP_DIM = D_MODEL // 16
SGU_WIDTH = SGU_GROUPS * SGU_GROUP_DIM
SGU_CHUNK = 128

N_BRANCH = 3
IN_SPLIT_SIZES = (POOL_WIDTH, 2 * MLSTM_WIDTH, MLSTM_WIDTH, MLSTM_WIDTH, 4 * MLSTM_HEADS, 2 * SGU_WIDTH, N_BRANCH * D_MODEL)
IN_COLS = sum(IN_SPLIT_SIZES)
MLSTM_GATE_OFF = POOL_WIDTH + 4 * MLSTM_WIDTH

N_EXPERTS = 32
TOP_K = 4
D_FF = D_MODEL
SWIGLU_LIMIT = 7.0
SWIGLU_ALPHA = 1.702

LN_EPS = 1e-5
HEAD_NORM_EPS = 1e-6
DEEPNORM_ALPHA = (2 * DEPTH) ** 0.25
DEEPNORM_BETA = (8 * DEPTH) ** -0.25

kernel_name = 'pool_mlstm_sgu_moe_flow_block'


def layer_norm_plain(x):
    xf = x.astype(jnp.float32)
    mu = xf.mean(-1, keepdims=True)
    var = jnp.square(xf - mu).mean(-1, keepdims=True)
    return ((xf - mu) * lax.rsqrt(var + LN_EPS)).astype(x.dtype)


def layer_norm(x, g, b):
    return layer_norm_plain(x) * g + b


def modulate(h, shift, scale):
    return h * (1 + scale) + shift


def split_in(z):
    idx = [int(s) for s in np.cumsum(IN_SPLIT_SIZES)[:-1]]
    return jnp.split(z, idx, axis=-1)


def centred_box_mean(x, window):
    length = x.shape[-2]
    xf = x.astype(jnp.float32)
    prefix = jnp.concatenate([jnp.zeros_like(xf[..., :1, :]), jnp.cumsum(xf, axis=-2)], axis=-2)
    t = jnp.arange(length)
    lo = jnp.clip(t - window // 2, 0, length)
    hi = jnp.clip(t + window // 2, 0, length)
    s = jnp.take(prefix, hi, axis=-2) - jnp.take(prefix, lo, axis=-2)
    cnt = (hi - lo).astype(jnp.float32)[:, None]
    return (s / cnt).astype(x.dtype)


def pool_mixer(xp, w_lin, scale):
    grp = xp.reshape(xp.shape[:-1] + (POOL_GROUPS, POOL_GROUP_DIM))
    pooled = jnp.stack([centred_box_mean(grp[..., g, :], w) for g, w in enumerate(POOL_WINDOWS)], axis=-2)
    y = jnp.einsum('...lgc,gcd->...lgd', pooled - grp, w_lin)
    return y.reshape(xp.shape) * scale


def dwconv_centred(x, w, b):
    k = w.shape[0]
    length = x.shape[1]
    left = k // 2
    xp = jnp.pad(x, ((0, 0), (left, k - 1 - left), (0, 0)))
    y = b
    for j in range(k):
        y = y + xp[:, j:j + length, :] * w[j]
    return y


def mlstm_prepare(qk, v, gates, conv_w, conv_b):
    qk = jax.nn.silu(dwconv_centred(qk, conv_w, conv_b))
    q, k = jnp.split(qk, 2, axis=-1)
    b, length, _ = v.shape

    def heads(a):
        return a.reshape(b, length, MLSTM_HEADS, MLSTM_HEAD_DIM).transpose(0, 2, 1, 3).astype(jnp.float32)

    g = gates.astype(jnp.float32).reshape(b, length, 4, MLSTM_HEADS).transpose(2, 0, 3, 1)
    return heads(q), heads(k) * MLSTM_HEAD_DIM ** -0.5, heads(v), g


def mlstm_chunk_scan(q, k, v, log_i, log_f, state):
    b, h, length, dh = q.shape
    nc = length // MLSTM_CHUNK
    lower = jnp.tril(jnp.ones((MLSTM_CHUNK, MLSTM_CHUNK), dtype=bool))

    def chunks(a):
        return jnp.moveaxis(a.reshape((b, h, nc, MLSTM_CHUNK) + a.shape[3:]), 2, 0)

    def step(carry, xs):
        c_mat, n_vec, m = carry
        qc, kc, vc, ic, fc = xs
        bcum = jnp.cumsum(fc, axis=-1)
        dmat = jnp.where(lower, bcum[..., :, None] - bcum[..., None, :] + ic[..., None, :], -jnp.inf)
        inter = bcum + m[..., None]
        m_t = jnp.maximum(inter, dmat.max(-1))
        decay = jnp.exp(inter - m_t)
        scores = jnp.einsum('bhtd,bhsd->bhts', qc, kc) * jnp.exp(dmat - m_t[..., None])
        num = decay[..., None] * jnp.einsum('bhvk,bhtk->bhtv', c_mat, qc) + jnp.einsum('bhts,bhsv->bhtv', scores, vc)
        den = decay * jnp.einsum('bhk,bhtk->bht', n_vec, qc) + scores.sum(-1)
        out = num / jnp.maximum(jnp.abs(den), jnp.exp(-m_t))[..., None]
        total = bcum[..., -1]
        wlog = total[..., None] - bcum + ic
        m_new = jnp.maximum(total + m, wlog.max(-1))
        a = jnp.exp(total + m - m_new)
        wk = jnp.exp(wlog - m_new[..., None])
        c_new = a[..., None, None] * c_mat + jnp.einsum('bhsv,bhsk->bhvk', vc * wk[..., None], kc)
        n_new = a[..., None] * n_vec + jnp.einsum('bhs,bhsk->bhk', wk, kc)
        return (c_new, n_new, m_new), out

    state, hs = lax.scan(step, state, tuple(chunks(a) for a in (q, k, v, log_i, log_f)))
    return jnp.moveaxis(hs, 0, 2).reshape(b, h, length, dh), state


def mlstm_bidirectional(ctx_in, lat_in):
    qc, kc, vc, gc = ctx_in
    ql, kl, vl, gl = lat_in
    b = qc.shape[0]
    zero = (jnp.zeros((b, MLSTM_HEADS, MLSTM_HEAD_DIM, MLSTM_HEAD_DIM), jnp.float32),
            jnp.zeros((b, MLSTM_HEADS, MLSTM_HEAD_DIM), jnp.float32),
            jnp.zeros((b, MLSTM_HEADS), jnp.float32))
    ls = jax.nn.log_sigmoid
    hc_f, st_f = mlstm_chunk_scan(qc, kc, vc, gc[0], ls(gc[1]), zero)
    hl_f, _ = mlstm_chunk_scan(ql, kl, vl, gl[0], ls(gl[1]), st_f)
    fs = lambda a: jnp.flip(a, axis=2)
    fg = lambda a: jnp.flip(a, axis=-1)
    hc_b, st_b = mlstm_chunk_scan(fs(qc), fs(kc), fs(vc), fg(gc[2]), ls(fg(gc[3])), zero)
    hl_b, _ = mlstm_chunk_scan(fs(ql), fs(kl), fs(vl), fg(gl[2]), ls(fg(gl[3])), st_b)
    return hc_f + fs(hc_b), hl_f + fs(hl_b)


def mlstm_output(h, o, norm_g):
    b, _, length, _ = h.shape
    mu = h.mean(-1, keepdims=True)
    var = jnp.square(h - mu).mean(-1, keepdims=True)
    hn = ((h - mu) * lax.rsqrt(var + HEAD_NORM_EPS)).transpose(0, 2, 1, 3).reshape(b, length, MLSTM_WIDTH)
    return (hn * norm_g * jax.nn.sigmoid(o.astype(jnp.float32))).astype(o.dtype)


def sgu_mixer(uv, ln_g, ln_b, w_s, b_s):
    u, v = jnp.split(jax.nn.gelu(uv, approximate=False), 2, axis=-1)
    v = layer_norm(v, ln_g, ln_b)
    b, length, _ = v.shape
    vc = v.reshape(b, length // SGU_CHUNK, SGU_CHUNK, SGU_GROUPS, SGU_GROUP_DIM)
    mixed = jnp.einsum('gts,bnsgc->bntgc', w_s, vc) + b_s.T[:, :, None]
    return u * mixed.reshape(b, length, SGU_WIDTH)


def branch_merge(pool_o, mlstm_o, sgu_o, gate_pre, w_bp, w_bm, w_bs, w_o):
    g = jax.nn.sigmoid(gate_pre.astype(jnp.float32)).astype(pool_o.dtype)
    g_p, g_m, g_s = jnp.split(g, N_BRANCH, axis=-1)
    y = g_p * (pool_o @ w_bp) + g_m * (mlstm_o @ w_bm) + g_s * (sgu_o @ w_bs)
    return y @ w_o


def moe_ffn(h, w_router, b_router, w_gate_up, b_gate_up, w_down, b_down):
    logits = (h @ w_router + b_router).astype(jnp.float32)
    top_val, top_idx = lax.top_k(logits, TOP_K)
    probs = jax.nn.softmax(top_val, axis=-1)
    combine = jnp.einsum('nk,nke->en', probs, jax.nn.one_hot(top_idx, N_EXPERTS, dtype=jnp.float32)).astype(h.dtype)

    def expert(acc, xs):
        wgu, bgu, wd, bd, cw = xs
        gate, up = jnp.split(h @ wgu + bgu, 2, axis=-1)
        gate = jnp.minimum(gate, SWIGLU_LIMIT)
        up = jnp.clip(up, -SWIGLU_LIMIT, SWIGLU_LIMIT)
        glu = gate * jax.nn.sigmoid(SWIGLU_ALPHA * gate)
        y = ((up + 1) * glu) @ wd + bd
        return acc + cw[:, None] * y, None

    out, _ = lax.scan(expert, jnp.zeros_like(h), (w_gate_up, b_gate_up, w_down, b_down, combine))
    return out


def setup_inputs(seed: int = 0) -> dict:
    key = jax.random.key(seed)
    ks = iter(jax.random.split(key, 40))

    def nrm(shape, scale):
        return scale * jax.random.normal(next(ks), shape, jnp.float32)

    L, D, E, F = DEPTH, D_MODEL, N_EXPERTS, D_FF
    f_bias = jnp.linspace(3.0, 6.0, MLSTM_HEADS, dtype=jnp.float32)
    b_in = nrm((L, IN_COLS), 0.02)
    b_in = b_in.at[:, MLSTM_GATE_OFF + MLSTM_HEADS:MLSTM_GATE_OFF + 2 * MLSTM_HEADS].add(f_bias)
    b_in = b_in.at[:, MLSTM_GATE_OFF + 3 * MLSTM_HEADS:MLSTM_GATE_OFF + 4 * MLSTM_HEADS].add(f_bias)
    return {
        'x': nrm((BATCH, SEQ, D), 1.0),
        'c': nrm((BATCH, D), 1.0),
        'ctx': nrm((BATCH, CTX_LEN, D), 1.0),
        'c_ctx': nrm((D,), 1.0),
        'w_ada': nrm((L, D, 6 * D), 0.5 * D ** -0.5),
        'b_ada': nrm((L, 6 * D), 0.02),
        'w_in': nrm((L, D, IN_COLS), D ** -0.5),
        'b_in': b_in,
        'pool_w': nrm((L, POOL_GROUPS, POOL_GROUP_DIM, POOL_GROUP_DIM), POOL_GROUP_DIM ** -0.5),
        'pool_scale': 1.0 + nrm((L, POOL_WIDTH), 0.02),
        'qk_conv_w': nrm((L, QK_CONV_WIDTH, 2 * MLSTM_WIDTH), QK_CONV_WIDTH ** -0.5),
        'qk_conv_b': nrm((L, 2 * MLSTM_WIDTH), 0.02),
        'mlstm_norm_g': 1.0 + nrm((L, MLSTM_WIDTH), 0.02),
        'sgu_ln_g': 1.0 + nrm((L, SGU_WIDTH), 0.02),
        'sgu_ln_b': nrm((L, SGU_WIDTH), 0.02),
        'sgu_w': nrm((L, SGU_GROUPS, SGU_CHUNK, SGU_CHUNK), SGU_CHUNK ** -0.5),
        'sgu_b': 1.0 + nrm((L, SGU_GROUPS, SGU_CHUNK), 0.02),
        'w_br_pool': nrm((L, POOL_WIDTH, D), POOL_WIDTH ** -0.5),
        'w_br_mlstm': nrm((L, MLSTM_WIDTH, D), MLSTM_WIDTH ** -0.5),
        'w_br_sgu': nrm((L, SGU_WIDTH, D), SGU_WIDTH ** -0.5),
        'w_out': nrm((L, D, D), DEEPNORM_BETA * D ** -0.5),
        'ln1_g': 1.0 + nrm((L, D), 0.02),
        'ln1_b': nrm((L, D), 0.02),
        'w_router': nrm((L, D, E), D ** -0.5),
        'b_router': nrm((L, E), 0.01),
        'w_gate_up': nrm((L, E, D, 2 * F), D ** -0.5),
        'b_gate_up': nrm((L, E, 2 * F), 0.02),
        'w_down': nrm((L, E, F, D), DEEPNORM_BETA * F ** -0.5),
        'b_down': nrm((L, E, D), 0.02),
        'ln2_g': 1.0 + nrm((L, D), 0.02),
        'ln2_b': nrm((L, D), 0.02),
    }


def reference(x, c, ctx, c_ctx, w_ada, b_ada, w_in, b_in, pool_w, pool_scale, qk_conv_w, qk_conv_b,
              mlstm_norm_g, sgu_ln_g, sgu_ln_b, sgu_w, sgu_b, w_br_pool, w_br_mlstm, w_br_sgu, w_out,
              ln1_g, ln1_b, w_router, b_router, w_gate_up, b_gate_up, w_down, b_down, ln2_g, ln2_b):
    B, S, D = x.shape
    rows = S // GRID_W
    xl, xc = x, ctx
    for i in range(DEPTH):
        last = i == DEPTH - 1
        sh1_l, sc1_l, g1_l, sh2_l, sc2_l, g2_l = [
            m[:, None, :] for m in jnp.split(jax.nn.silu(c) @ w_ada[i] + b_ada[i], 6, axis=-1)]
        sh1_c, sc1_c, g1_c, sh2_c, sc2_c, g2_c = jnp.split(jax.nn.silu(c_ctx) @ w_ada[i] + b_ada[i], 6, axis=-1)

        z_l = modulate(layer_norm_plain(xl), sh1_l, sc1_l) @ w_in[i] + b_in[i]
        z_c = modulate(layer_norm_plain(xc), sh1_c, sc1_c) @ w_in[i] + b_in[i]
        p_l, qk_l, v_l, o_l, gt_l, uv_l, mg_l = split_in(z_l)
        p_c, qk_c, v_c, o_c, gt_c, uv_c, mg_c = split_in(z_c)

        hm_c, hm_l = mlstm_bidirectional(
            mlstm_prepare(qk_c, v_c, gt_c, qk_conv_w[i], qk_conv_b[i]),
            mlstm_prepare(qk_l, v_l, gt_l, qk_conv_w[i], qk_conv_b[i]))
        merge_w = (w_br_pool[i], w_br_mlstm[i], w_br_sgu[i], w_out[i])
        moe_w = (w_router[i], b_router[i], w_gate_up[i], b_gate_up[i], w_down[i], b_down[i])

        y_l = branch_merge(
            pool_mixer(p_l.reshape(B, rows, GRID_W, POOL_WIDTH), pool_w[i], pool_scale[i]).reshape(B, S, POOL_WIDTH),
            mlstm_output(hm_l, o_l, mlstm_norm_g[i]),
            sgu_mixer(uv_l, sgu_ln_g[i], sgu_ln_b[i], sgu_w[i], sgu_b[i]),
            mg_l, *merge_w)
        xl = layer_norm(DEEPNORM_ALPHA * xl + g1_l * y_l, ln1_g[i], ln1_b[i])
        h_l = modulate(layer_norm_plain(xl), sh2_l, sc2_l).reshape(B * S, D)

        if last:
            f_l = moe_ffn(h_l, *moe_w)
        else:
            y_c = branch_merge(
                pool_mixer(p_c, pool_w[i], pool_scale[i]),
                mlstm_output(hm_c, o_c, mlstm_norm_g[i]),
                sgu_mixer(uv_c, sgu_ln_g[i], sgu_ln_b[i], sgu_w[i], sgu_b[i]),
                mg_c, *merge_w)
            xc = layer_norm(DEEPNORM_ALPHA * xc + g1_c * y_c, ln1_g[i], ln1_b[i])
            h_c = modulate(layer_norm_plain(xc), sh2_c, sc2_c).reshape(-1, D)
            f_all = moe_ffn(jnp.concatenate([h_l, h_c], axis=0), *moe_w)
            f_l, f_c = f_all[:B * S], f_all[B * S:]
            xc = layer_norm(DEEPNORM_ALPHA * xc + g2_c * f_c.reshape(xc.shape), ln2_g[i], ln2_b[i])
        xl = layer_norm(DEEPNORM_ALPHA * xl + g2_l * f_l.reshape(B, S, D), ln2_g[i], ln2_b[i])
    return xl
```

```python
import math
from contextlib import ExitStack
import numpy as np
import ml_dtypes
import concourse.bass as bass
import concourse.mybir as mybir
from concourse.bass_utils import run_bass_kernel_spmd

F32 = mybir.dt.float32
BF16 = mybir.dt.bfloat16
ALU = mybir.AluOpType
AF = mybir.ActivationFunctionType
AX = mybir.AxisListType

D = 1024
CTX = 256
GRID_W = 64
POOL_W = (2, 4, 8, 16)
IN_COLS = 5904
NTM = 4880
LN_EPS = 1e-5
HN_EPS = 1e-6
ALPHA = 8.0 ** 0.25
SEM_LIMIT = 30000
NEG = -1.0e30


class Sched:
    def __init__(self, nc, stack):
        self.nc = nc
        self.stack = stack
        self.engs = {"pe": nc.tensor, "dve": nc.vector, "act": nc.scalar, "pool": nc.gpsimd, "sp": nc.sync}
        self.ops = {k: [] for k in self.engs}
        self.sems = []
        self.cur = {}
        self.covered = {}
        self.lastw = {}
        self.reads = {}
        self.dma_n = {}
        self.dma_slots = {}
        self.NDS = 8
        self.nsem = 0

    def new_sem(self, owner=None):
        s = self.stack.enter_context(self.nc.semaphore("s%d" % self.nsem))
        self.nsem += 1
        self.sems.append(s)
        self.owner = getattr(self, "owner", {})
        self.owner[len(self.sems) - 1] = owner
        return len(self.sems) - 1

    def _deps(self, eng, reads, writes):
        deps = set()
        for t in list(reads) + list(writes):
            e = self.lastw.get(t)
            if e is not None:
                deps.add(e)
        for t in writes:
            for e in self.reads.get(t, ()):
                deps.add(e)
        waits = []
        for (si, v) in sorted(deps):
            if eng == "pe" and self.owner.get(si) == "pe":
                continue
            if self.covered.get((eng, si), 0) >= v:
                continue
            self.covered[(eng, si)] = v
            waits.append((si, v))
        return waits

    def _record(self, ev, reads, writes):
        for t in writes:
            self.lastw[t] = ev
            self.reads[t] = []
        for t in reads:
            self.reads.setdefault(t, []).append(ev)

    def op(self, eng, fn, reads=(), writes=(), sig=True):
        waits = self._deps(eng, reads, writes)
        if eng not in self.cur:
            self.cur[eng] = [self.new_sem(eng), 0]
        c = self.cur[eng]
        ev = (c[0], c[1] + 1)
        if sig:
            c[1] += 1
        self.ops[eng].append((waits, fn, (c[0], 1) if sig else None))
        self._record(ev, reads, writes)
        if sig and c[1] >= SEM_LIMIT:
            self.cur[eng] = [self.new_sem(eng), 0]
        return ev

    def dma(self, q, out, in_, reads=(), writes=(), method="dma_start", **kw):
        n = self.dma_n.get(q, 0)
        self.dma_n[q] = n + 1
        slot = n % self.NDS
        key = (q, slot)
        st = self.dma_slots.get(key)
        if st is None or st[1] + 16 > SEM_LIMIT:
            prev = st
            st = [self.new_sem("dma"), 0]
            self.dma_slots[key] = st
            waits0 = [(prev[0], prev[1])] if prev is not None else []
        else:
            waits0 = [(st[0], st[1])] if st[1] > 0 else []
        waits = self._deps(q, reads, writes)
        for (si, v) in waits0:
            if self.covered.get((q, si), 0) < v:
                self.covered[(q, si)] = v
                waits.append((si, v))
        st[1] += 16
        ev = (st[0], st[1])
        self.ops[q].append((waits, (method, (), dict(out=out, in_=in_, **kw)), (st[0], 16)))
        self._record(ev, reads, writes)
        return ev

    def barrier(self):
        evs = []
        for eng, c in self.cur.items():
            if c[1] > 0:
                evs.append((c[0], c[1]))
            elif c[0] > 0 and self.owner.get(c[0] - 0) == eng:
                for si in range(c[0] - 1, -1, -1):
                    if self.owner.get(si) == eng:
                        evs.append((si, SEM_LIMIT))
                        break
        for key, st in self.dma_slots.items():
            if st[1] > 0:
                evs.append((st[0], st[1]))
        for eng in self.engs:
            w = []
            for (si, v) in evs:
                if self.covered.get((eng, si), 0) < v:
                    self.covered[(eng, si)] = v
                    w.append((si, v))
            if w:
                self.ops[eng].append((w, None, None))

    def final_wait(self, eng, evs):
        self.ops[eng].append(([(si, v) for (si, v) in evs], None, None))

    def emit(self):
        nc = self.nc
        sems = self.sems
        with nc.Block() as block:
            def mk(name):
                def body(e):
                    for (waits, fn, inc) in self.ops[name]:
                        for (si, v) in waits:
                            e.wait_ge(sems[si], v)
                        if fn is None:
                            continue
                        ins = getattr(e, fn[0])(*fn[1], **fn[2])
                        if inc is not None:
                            ins.then_inc(sems[inc[0]], inc[1])
                return body
            block.tensor(mk("pe"))
            block.vector(mk("dve"))
            block.scalar(mk("act"))
            block.gpsimd(mk("pool"))
            block.sync(mk("sp"))


def MK(m, *a, **k):
    return (m, a, k)


class PerLayer:
    def __init__(self, ts):
        self.ts = ts

    def __getitem__(self, key):
        if isinstance(key, tuple):
            return self.ts[key[0]][key[1:]]
        return self.ts[key]


def make_consts():
    I = np.eye(128, dtype=np.float32)
    s = np.arange(128)[:, None]
    t = np.arange(128)[None, :]
    c = {}
    c["ident"] = I
    c["ones"] = np.ones((128, 128), np.float32)
    c["triF"] = (s <= t).astype(np.float32)
    c["triB"] = (s >= t).astype(np.float32)
    c["triS"] = (s < t).astype(np.float32)
    c["jts"] = np.repeat((np.arange(128, dtype=np.float32) * 512.0)[None, :], 128, axis=0)
    cg = np.zeros((128, 128), np.float32)
    cg[:, 0:8] = np.arange(8)[None, :] * 128.0 + np.arange(128)[:, None]
    c["cg"] = cg
    c["selF"] = np.repeat((s == 127).astype(np.float32), 128, axis=1)
    c["selB"] = np.repeat((s == 0).astype(np.float32), 128, axis=1)
    mF = np.where(t <= s, 0.0, NEG).astype(np.float32)
    mB = np.where(t >= s, 0.0, NEG).astype(np.float32)
    c["cmF"] = mF
    c["cmB"] = mB
    dF = np.where(s <= t, 0.0, NEG).astype(np.float32)
    dB = np.where(s >= t, 0.0, NEG).astype(np.float32)
    c["dmF"] = dF
    c["dmB"] = dB

    def band(length, w):
        tt = np.arange(length)
        lo = np.clip(tt - w // 2, 0, length)
        hi = np.clip(tt + w // 2, 0, length)
        P = np.zeros((length, length), np.float32)
        for a in range(length):
            P[a, lo[a]:hi[a]] = 1.0 / (hi[a] - lo[a])
        return P - np.eye(length, dtype=np.float32)
    pl = []
    for w in POOL_W:
        P64 = band(64, w)
        P = np.zeros((128, 128), np.float32)
        P[:64, :64] = P64
        P[64:, 64:] = P64
        pl.append(P.T.copy())
    c["poolL"] = np.concatenate(pl, axis=1)
    pc = []
    for w in POOL_W:
        P = band(256, w).T
        for i in range(2):
            for j in range(2):
                pc.append(P[j * 128:(j + 1) * 128, i * 128:(i + 1) * 128].copy())
    c["poolC"] = np.concatenate(pc, axis=1)
    return c


CONST_ORDER = ["ident", "ones", "triF", "triB", "selF", "selB", "cmF", "cmB", "dmF", "dmB", "triS", "jts", "cg", "poolL", "poolC"]


def const_offsets():
    c = make_consts()
    off = {}
    o = 0
    for k in CONST_ORDER:
        off[k] = (o, c[k].shape[1])
        o += c[k].shape[1]
    return off, o


def build(cfg):
    NB, S, DEPTH, E, NCORES = cfg["NB"], cfg["S"], cfg["DEPTH"], cfg["E"], cfg["NCORES"]
    LT = CTX + S
    NT = LT // 128
    NTOK = NB * LT
    coff, CW = const_offsets()
    nc = bass.Bass("TRN2", target_bir_lowering=False)
    stack = ExitStack()
    sc = Sched(nc, stack)

    def dram_in(name, shape, dt=F32):
        return nc.dram_tensor(name, list(shape), dt, kind="ExternalInput").ap()

    def dram(name, shape, dt=F32):
        return nc.dram_tensor(name, list(shape), dt).ap()

    def sb(name, shape, dt=F32):
        return stack.enter_context(nc.sbuf_tensor(name, list(shape), dt))

    ARF, ARB = 16128, 32768
    arena = {}
    phase_off = {"f": 0, "b": 0}

    def new_phase():
        sc.barrier()
        phase_off["f"] = 0
        phase_off["b"] = 0

    def asb(name, shape, dt=F32):
        if "f" not in arena:
            arena["f"] = sb("arenaF", [128, ARF], F32)
            arena["b"] = sb("arenaB", [128, ARB], BF16)
        key = "f" if dt == F32 else "b"
        n = 1
        for d_ in shape[1:]:
            n *= d_
        n = (n + 15) // 16 * 16
        o = phase_off[key]
        phase_off[key] = o + n
        assert phase_off[key] <= (ARF if key == "f" else ARB), (name, key, phase_off[key])
        nn = 1
        for d_ in shape[1:]:
            nn *= d_
        v = arena[key][0:shape[0], o:o + nn]
        if len(shape) == 3:
            v = v.rearrange("p (a b) -> p a b", a=shape[1])
        elif len(shape) == 4:
            v = v.rearrange("p (a b c) -> p a b c", a=shape[1], b=shape[2])
        return v

    def ps(name, shape, dt=F32):
        return stack.enter_context(nc.psum_tensor(name, list(shape), dt))

    xin = dram_in("xin", [NTOK, D])
    cT_in = dram_in("cT", [128, 8, NB + 1])
    consts_in = dram_in("consts", [128, CW])
    onehot_in = dram_in("onehot", [128, 8])
    SH = NCORES
    w_ada_s = dram_in("w_ada", [DEPTH, D // SH, 6 * D])
    w_in_s = dram_in("w_in", [DEPTH, D // SH, IN_COLS])
    w_br_s = dram_in("w_br", [DEPTH, 1024 // SH, D])
    w_out_s = dram_in("w_out", [DEPTH, D // SH, D])
    w_gu_s = dram_in("w_gu", [DEPTH, E * D // SH, 2 * D])
    w_dn_s = dram_in("w_dn", [DEPTH, E * 128 // SH, 8 * D])
    w_router = dram_in("w_router", [DEPTH, 128, 8, E])
    rows_in = dram_in("rows", [DEPTH, 1, 17408])
    cols_in = dram_in("cols", [DEPTH, 128, 256])
    pool_w_in = dram_in("pool_w", [DEPTH, 64, 4, 64])
    sgu_wT_in = dram_in("sgu_wT", [DEPTH, 128, 4, 128])
    bgu_in = [dram_in("bgu%d" % l_, [E * 128, 16]) for l_ in range(DEPTH)]
    bdn_in = dram_in("bdn", [DEPTH, E, D])
    out = nc.dram_tensor("out", [NB * S, D], F32, kind="ExternalOutput").ap()

    R_BADA, R_BIN, R_LN1G, R_LN1B, R_LN2G, R_LN2B, R_NG, R_SLG, R_SLB, R_BR = 0, 6144, 11024, 12048, 13072, 14096, 15120, 15632, 15888, 16144
    C_BADA, C_BINQK, C_CW, C_CB, C_PSC, C_BS = 0, 48, 56, 80, 88, 92

    xs = dram("xs", [NTOK, D])
    wb_ada = PerLayer([dram("wb_ada_%d" % l_, [D, 6 * D], BF16) for l_ in range(DEPTH)])
    stg_d = {}
    wb_in = PerLayer([dram("wb_in_%d" % l_, [D, IN_COLS], BF16) for l_ in range(DEPTH)])
    wb_br = PerLayer([dram("wb_br_%d" % l_, [1024, D], BF16) for l_ in range(DEPTH)])
    wb_out = PerLayer([dram("wb_out_%d" % l_, [D, D], BF16) for l_ in range(DEPTH)])
    wb_gu = PerLayer([dram("wb_gu_%d" % l_, [E * D, 2 * D], BF16) for l_ in range(DEPTH)])
    wb_dn = PerLayer([dram("wb_dn_%d" % l_, [E * 128, 8 * D], BF16) for l_ in range(DEPTH)])
    zt = dram("zt", [NTOK, NTM])
    qkT = dram("qkT", [8, 128, NTOK])
    qkcd = dram("qkcd", [8, 128, NTOK], BF16)
    hb = dram("hb", [NTOK, 512])
    TS = 512
    NTILE = (4 * NTOK + TS - 1) // TS + E
    NSLOT = NTILE * TS
    Hs = dram("Hs", [NSLOT, D], BF16)
    Yd = dram("Yd", [NSLOT, D])
    h2d = dram("h2d", [NTOK, D], BF16)
    rankd = dram("rankd", [NTOK, E])
    ohd = dram("ohd", [NTOK, 4 * E])
    cwd = dram("cwd", [NTOK, E])
    cwTd = dram("cwTd", [E, NTOK])

    NCF = 13 * 128
    cf = sb("cf", [128, NCF])
    cb = sb("cb", [128, CW], BF16)
    dummy = sb("dummyj", [1, 16])
    sc.dma("sp", cf[:], consts_in[:, 0:NCF], writes=["cf"])

    def CF(name, a=0, n=None):
        o, w = coff[name]
        n = w - a if n is None else n
        assert o + a + n <= NCF
        return cf[:, o + a:o + a + n]

    def CB(name, a=0, n=None):
        o, w = coff[name]
        n = w - a if n is None else n
        return cb[:, o + a:o + a + n]

    def join(reads, tag):
        sc.op("pool", MK("memset", dummy[:], 0.0), reads=list(reads) + ["dummy"], writes=["dummy", tag])

    stg = [asb("wst%d" % i, [128, 1024]) for i in range(2)]
    stb = [asb("wsb%d" % i, [128, 1024], BF16) for i in range(2)]
    oh = asb("oh", [128, 8])
    sc.dma("sp", oh, onehot_in, writes=["oh"])
    ccnt = [0]
    for c0 in range(0, CW, 1024):
        c1 = min(CW, c0 + 1024)
        k = ccnt[0] % 2
        ccnt[0] += 1
        sc.dma("sp", stg[k][:, 0:c1 - c0], consts_in[:, c0:c1], writes=["wst%d" % k])
        sc.op("dve", MK("tensor_copy", cb[:, c0:c1], stg[k][:, 0:c1 - c0]), reads=["wst%d" % k], writes=["cb"])
    SH = NCORES
    wlist = [("ada", w_ada_s, wb_ada, D, 6 * D), ("in", w_in_s, wb_in, D, IN_COLS), ("br", w_br_s, wb_br, 1024, D),
             ("out", w_out_s, wb_out, D, D), ("gu", w_gu_s, wb_gu, E * D, 2 * D), ("dn", w_dn_s, wb_dn, E * 128, 8 * D)]
    WTAG = {}
    for l in range(DEPTH):
        for (wn, src, dst, rows, cols) in wlist:
            rs = rows // SH
            if SH > 1:
                if wn not in stg_d:
                    stg_d[wn] = dram("stg_" + wn, [rows, cols], BF16)
                tgt = stg_d[wn]
            else:
                tgt = dst[l]
            prev_dep = [("W", wn, l - 1)] if (SH > 1 and l > 0) else []
            chunk_tags = []
            for r0 in range(0, rs, 128):
                pp = min(128, rs - r0)
                for c0 in range(0, cols, 1024):
                    c1 = min(cols, c0 + 1024)
                    n = c1 - c0
                    k = ccnt[0] % 2
                    ccnt[0] += 1
                    sc.dma("sp", stg[k][0:pp, 0:n], src[l, r0:r0 + pp, c0:c1], writes=["wst%d" % k])
                    for r in range(SH):
                        eng = "dve" if (ccnt[0] + r) % 2 == 0 else "pool"
                        if SH == 1:
                            sc.op(eng, MK("tensor_copy", stb[k][0:pp, 0:n], stg[k][0:pp, 0:n]), reads=["wst%d" % k], writes=["wsb%d" % k])
                        else:
                            sc.op(eng, MK("tensor_scalar", stb[k][0:pp, 0:n], stg[k][0:pp, 0:n], oh[0:pp, r:r + 1], None, ALU.mult),
                                  reads=["wst%d" % k, "oh"], writes=["wsb%d" % k])
                        tg = ("Wc", wn, l, r, r0, c0)
                        chunk_tags.append(tg)
                        sc.dma("pool", tgt[r * rs + r0:r * rs + r0 + pp, c0:c1], stb[k][0:pp, 0:n], reads=["wsb%d" % k] + prev_dep, writes=[tg])
            if SH > 1:
                join(chunk_tags, ("Ws", wn, l))
                step = max(128, (32 * 1024 * 1024) // (cols * 2) // 128 * 128)
                ar_tags = []
                for r0 in range(0, rows, step):
                    r1 = min(rows, r0 + step)
                    tg = ("Wa", wn, l, r0)
                    ar_tags.append(tg)
                    sc.op("pool", MK("collective_compute", "AllReduce", ALU.add, replica_groups=[list(range(NCORES))],
                                                                                           ins=[tgt[r0:r1, :].opt()], outs=[dst[l, r0:r1, :].opt()]),
                          reads=[("Ws", wn, l)], writes=[tg])
                join(ar_tags, ("W", wn, l))
            else:
                join(chunk_tags, ("W", wn, l))

    cols_sb = sb("cols_sb", [128, 256])
    lnbc = sb("lnbc", [128, 4, D])
    smallbc = sb("smallbc", [128, 1024])
    gbc = sb("gbc", [128, 2 * (NB + 1), D])
    modc = sb("modc", [128, 32, NB + 1])
    scT = sb("scT", [128, 8, NB + 1])
    scTb = sb("scTb", [128, 8, NB + 1], BF16)
    screp = sb("screp", [128, 8, NB + 1, 128], BF16)
    brow = sb("brow", [1, 32])
    carry = sb("carry", [128, E])
    U32 = mybir.dt.uint32
    idxg_u = sb("idxg_u", [128, NTILE * 8], U32)
    idxd_u = sb("idxd_u", [128, NTILE], U32)
    pos4_u = [sb("pos4u%d" % i, [128, 4], U32) for i in range(2)]
    NTT = NTOK // 128
    pos4_all = sb("pos4all", [128, NTT * 4], U32)
    cw4_all = sb("cw4all", [128, NTT * 4])
    xt = [sb("xt%d" % i, [128, D]) for i in range(2)]
    stats = sb("stats", [128, 2, 6])
    mv = sb("mv", [128, 2])
    rstd = sb("rstd", [128, 1])
    st2 = sb("st2", [128, 2, 6])
    mv2 = sb("mv2", [128, 2])
    rs2 = sb("rs2", [128, 1])

    psA = [ps("psA%d" % i, [128, 512]) for i in range(2)]
    psT = [ps("psT0", [128, 1024], BF16)]
    psS_ = ps("psS", [128, 512])
    psS = [psS_[:, 0:256], psS_[:, 256:512]]
    psN = ps("psN", [128, 4, 256])
    psF = ps("psF", [128, 1024])
    rr = {"a": 0, "s": 0}

    def bankA():
        rr["a"] = (rr["a"] + 1) % 2
        return rr["a"]

    def bankT():
        return 0

    def bankS():
        rr["s"] = (rr["s"] + 1) % 2
        return rr["s"]

    def mm(o, l, r, start, stop, reads, writes):
        sc.op("pe", MK("matmul", o, l, r, start=start, stop=stop), reads=reads, writes=writes, sig=stop)

    def tp(o, i, ident, reads, writes, sig=True):
        sc.op("pe", MK("transpose", o, i, ident), reads=reads, writes=writes, sig=sig)

    def layer_norm_stats(src, width, tag_r, st=stats, mv_=mv, rstd_=rstd, eps=LN_EPS, tagp=""):
        nchunk = max(1, width // 512)
        w = min(512, width)
        for j in range(nchunk):
            sc.op("dve", MK("bn_stats", st[:, j, :], src[:, j * w:(j + 1) * w]), reads=tag_r, writes=[tagp + "stats%d" % j])
        sc.op("dve", MK("bn_aggr", mv_[:], st[:, 0:nchunk, :].rearrange("p a b -> p (a b)")), reads=[tagp + "stats%d" % j for j in range(nchunk)], writes=[tagp + "mv"])
        sc.op("dve", MK("tensor_scalar", rstd_[:], mv_[:, 1:2], eps, None, ALU.add), reads=[tagp + "mv"], writes=[tagp + "rstd"])
        sc.op("act", MK("activation", rstd_[:], rstd_[:], AF.Sqrt), reads=[tagp + "rstd"], writes=[tagp + "rstd"])
        sc.op("dve", MK("reciprocal", rstd_[:], rstd_[:]), reads=[tagp + "rstd"], writes=[tagp + "rstd"])

    R_BADA, R_BIN, R_LN1G, R_LN1B, R_LN2G, R_LN2B, R_NG, R_BR = 0, 6144, 12048, 13072, 14096, 15120, 16144, 17168
    C_BADA, C_BINQK, C_CW, C_CB, C_PSC, C_BS = 0, 48, 56, 80, 88, 92

    for l in range(DEPTH):
        last = (l == DEPTH - 1)
        xsrc = xin if l == 0 else xs
        WL = lambda wn: ("W", wn, l)
        new_phase()
        rowst = asb("rowst", [1, 2048])
        rows_bf = asb("rows_bf", [1, 6144], BF16)
        wada_sb = [asb("wada%d" % i, [128, 8, 512], BF16) for i in range(2)]
        for r0_ in range(0, 6144, 2048):
            sc.dma("sp", rowst, rows_in[l, :, R_BADA + r0_:R_BADA + r0_ + 2048], writes=["rowst"])
            sc.op("pool", MK("tensor_copy", rows_bf[:, r0_:r0_ + 2048], rowst), reads=["rowst"], writes=["rows_bf"])
        sc.dma("sp", brow[:, 0:E], rows_in[l, :, R_BR:R_BR + E], writes=["brow"])
        sc.dma("sp", cols_sb[:], cols_in[l], writes=["cols_sb"])
        for i, off in enumerate([R_LN1G, R_LN1B, R_LN2G, R_LN2B]):
            sc.dma("sp", lnbc[:, i, :], rows_in[l, 0, off:off + D].partition_broadcast(128), writes=["lnbc"])
        sc.dma("sp", smallbc[:], rows_in[l, 0, R_NG:R_NG + 1024].partition_broadcast(128), writes=["smallbc"])
        if l == 0:
            sc.dma("sp", scT[:], cT_in, writes=["scT"])
            sc.op("act", MK("activation", scT[:], scT[:], AF.Silu), reads=["scT"], writes=["scT"])
            sc.op("dve", MK("tensor_copy", scTb[:], scT[:]), reads=["scT"], writes=["scTb"])
            sc.op("dve", MK("tensor_copy", screp[:], scTb[:].unsqueeze(3).to_broadcast([128, 8, NB + 1, 128])), reads=["scTb"], writes=["screp"])
        for cb_i in range(12):
            wa = wada_sb[cb_i % 2]
            wtag = "wada%d" % (cb_i % 2)
            sc.dma("sp", wa, wb_ada[l, :, cb_i * 512:(cb_i + 1) * 512].rearrange("(k p) c -> p k c", p=128), reads=[WL("ada")], writes=[wtag])
            blk = cb_i // 2
            if blk in (2, 5):
                for s_ in range(NB + 1):
                    b = bankA()
                    for k in range(8):
                        mm(psA[b][:, :], screp[:, k, s_, :], wa[:, k, :], k == 0, False, [wtag, "screp"], ["psA%d" % b])
                    mm(psA[b][:, :], CB("ones", 0, 128)[0:1, :], rows_bf[0:1, cb_i * 512:(cb_i + 1) * 512], False, True, ["cb", "rows_bf"], ["psA%d" % b])
                    gi = (0 if blk == 2 else 1) * (NB + 1) + s_
                    half = cb_i % 2
                    sc.op("act", MK("copy", gbc[:, gi, half * 512:(half + 1) * 512], psA[b][:, :]), reads=["psA%d" % b], writes=["gbc"])
            else:
                vi = {0: 0, 1: 1, 3: 2, 4: 3}[blk]
                for cc in range(4):
                    b = bankA()
                    for k in range(8):
                        mm(psA[b][:, 0:NB + 1], wa[:, k, cc * 128:(cc + 1) * 128], scTb[:, k, :], k == 0, k == 7, [wtag, "scTb"], ["psA%d" % b])
                    col = C_BADA + cb_i * 4 + cc
                    j = vi * 8 + (cb_i % 2) * 4 + cc
                    addc = 1.0 if vi in (1, 3) else 0.0
                    sc.op("dve", MK("tensor_scalar", modc[:, j, :], psA[b][:, 0:NB + 1], cols_sb[:, col:col + 1], addc, ALU.add, ALU.add),
                          reads=["psA%d" % b, "cols_sb"], writes=["modc"])

        new_phase()
        rowst = asb("rowst", [1, 2048])
        rows_bf = asb("rows_bf", [1, 6144], BF16)
        wqk_sb = asb("wqk_sb", [128, 8, 1024], BF16)
        wblk = [asb("wblk%d" % i, [128, 8, 512], BF16) for i in range(2)]
        xnb = asb("xnb", [128, D], BF16)
        hT = asb("hT", [128, 8, 128], BF16)
        zst = [asb("zst%d" % i, [128, 512]) for i in range(3)]
        qkst = [asb("qkst%d" % i, [128, 128]) for i in range(2)]
        for r0_ in range(0, 6144, 2048):
            sc.dma("sp", rowst, rows_in[l, :, R_BIN + r0_:R_BIN + r0_ + 2048], writes=["rowst"])
            sc.op("pool", MK("tensor_copy", rows_bf[:, r0_:r0_ + 2048], rowst), reads=["rowst"], writes=["rows_bf"])
        sc.dma("sp", wqk_sb, wb_in[l, :, 256:1280].rearrange("(k p) c -> p k c", p=128), reads=[WL("in")], writes=["wqk_sb"])

        def load_x(gt):
            k = gt % 2
            sc.dma("sp", xt[k][:], xsrc[gt * 128:(gt + 1) * 128, :], reads=[("xs", gt)], writes=["xt%d" % k])

        tiles = [(b_, i) for b_ in range(NB) for i in range(NT)]
        bounds = [256, 768, 1280, 1296, 1808, NTM]
        segs = []
        c0 = 0
        while c0 < NTM:
            nb_ = min(x_ for x_ in bounds if x_ > c0)
            c1 = min(c0 + 512, nb_)
            segs.append((c0, c1))
            c0 = c1
        load_x(0)
        for n_, (b_, i) in enumerate(tiles):
            gt = b_ * NT + i
            src_i = NB if i < 2 else b_
            if n_ + 1 < len(tiles):
                load_x(gt + 1)
            k = gt % 2
            layer_norm_stats(xt[k], D, ["xt%d" % k])
            sc.op("dve", MK("tensor_scalar", xnb, xt[k][:], mv[:, 0:1], rstd[:, 0:1], ALU.subtract, ALU.mult), reads=["xt%d" % k, "mv", "rstd"], writes=["xnb"])
            for kc in range(8):
                tp(psT[0][:, kc * 128:(kc + 1) * 128], xnb[:, kc * 128:(kc + 1) * 128], CB("ident"), ["xnb", "cb"], ["psT0"], sig=(kc == 7))
            for kc in range(8):
                sc.op("act", MK("activation", hT[:, kc, :], psT[0][:, kc * 128:(kc + 1) * 128], AF.Identity,
                                                                 bias=modc[:, 0 + kc, src_i:src_i + 1], scale=modc[:, 8 + kc, src_i:src_i + 1]),
                      reads=["psT0", "modc"], writes=["hT"])
            for si_, (c0, c1) in enumerate(segs):
                wc0 = c0 if c0 < 256 else c0 + 1024
                n = c1 - c0
                b = bankA()
                wk_ = si_ % 2
                sc.dma("sp", wblk[wk_][:, :, 0:n], wb_in[l, :, wc0:wc0 + n].rearrange("(k p) c -> p k c", p=128), reads=[WL("in")], writes=["wblk%d" % wk_])
                for kc in range(8):
                    mm(psA[b][:, 0:n], hT[:, kc, :], wblk[wk_][:, kc, 0:n], kc == 0, False, ["hT", "wblk%d" % wk_], ["psA%d" % b])
                mm(psA[b][:, 0:n], CB("ones", 0, 128)[0:1, :], rows_bf[0:1, wc0:wc0 + n], False, True, ["cb", "rows_bf"], ["psA%d" % b])
                zk = si_ % 3
                if 768 <= c0 < 1280 or c0 >= 1808:
                    f = AF.Sigmoid
                elif 1296 <= c0 < 1808:
                    f = AF.Gelu
                else:
                    f = None
                if f is not None:
                    sc.op("act", MK("activation", zst[zk][:, 0:n], psA[b][:, 0:n], f), reads=["psA%d" % b], writes=["zst%d" % zk])
                else:
                    sc.op("dve", MK("tensor_copy", zst[zk][:, 0:n], psA[b][:, 0:n]), reads=["psA%d" % b], writes=["zst%d" % zk])
                sc.dma("pool", zt[gt * 128:(gt + 1) * 128, c0:c1], zst[zk][:, 0:n], reads=["zst%d" % zk], writes=[("zt", gt, si_)])
            join([("zt", gt, si_) for si_ in range(len(segs))], ("zt", gt))
            for cc in range(8):
                b = bankA()
                for kc in range(8):
                    mm(psA[b][:, 0:128], wqk_sb[:, kc, cc * 128:(cc + 1) * 128], hT[:, kc, :], kc == 0, kc == 7, ["hT", "wqk_sb"], ["psA%d" % b])
                qk_ = cc % 2
                sc.op("dve", MK("tensor_scalar", qkst[qk_], psA[b][:, 0:128], cols_sb[:, C_BINQK + cc:C_BINQK + cc + 1], None, ALU.add),
                      reads=["psA%d" % b, "cols_sb"], writes=["qkst%d" % qk_])
                sc.dma("pool", qkT[cc, :, gt * 128:(gt + 1) * 128], qkst[qk_], reads=["qkst%d" % qk_], writes=[("qkT", gt, cc)])
            join([("qkT", gt, cc) for cc in range(8)], ("qkT", gt))

        new_phase()
        wbr_p = asb("wbr_p", [64, 4, D], BF16)
        wbr_m = asb("wbr_m", [128, 4, D], BF16)
        wbr_s = asb("wbr_s", [128, 2, D], BF16)
        wout_sb = asb("wout_sb", [128, 8, D], BF16)
        wr_sb = asb("wr_sb", [128, 8, E])
        poolw_f = asb("poolw_f", [64, 4, 64])
        poolw_b = asb("poolw_b", [64, 4, 64], BF16)
        sguw_f = asb("sguw_f", [128, 4, 128])
        sguw_b = asb("sguw_b", [128, 4, 128], BF16)
        sc.dma("sp", wbr_p, wb_br[l, 0:256, :].rearrange("(g p) c -> p g c", p=64), reads=[WL("br")], writes=["wbr_p"])
        sc.dma("sp", wbr_m, wb_br[l, 256:768, :].rearrange("(g p) c -> p g c", p=128), reads=[WL("br")], writes=["wbr_m"])
        sc.dma("sp", wbr_s, wb_br[l, 768:1024, :].rearrange("(g p) c -> p g c", p=128), reads=[WL("br")], writes=["wbr_s"])
        sc.dma("sp", wout_sb, wb_out[l].rearrange("(k p) c -> p k c", p=128), reads=[WL("out")], writes=["wout_sb"])
        sc.dma("sp", wr_sb, w_router[l], writes=["wr_sb"])
        sc.dma("sp", poolw_f, pool_w_in[l], writes=["poolw_f"])
        sc.op("dve", MK("tensor_copy", poolw_b, poolw_f), reads=["poolw_f"], writes=["poolw_b"])
        sc.dma("sp", sguw_f, sgu_wT_in[l], writes=["sguw_f"])
        sc.op("dve", MK("tensor_copy", sguw_b, sguw_f), reads=["sguw_f"], writes=["sguw_b"])

        sc.op("pool", MK("memset", carry[:], 0.0), writes=["carry"])
        CP = 1024 if S >= 1024 else S
        craw = asb("craw", [128, CP + 2])
        ctmp = asb("ctmp", [128, CP])
        cob = asb("cob", [128, CP], BF16)
        qkt = [asb("qkt%d" % i, [128, 8, 128], BF16) for i in range(2)]
        g16 = asb("g16", [128, 16])
        vt = asb("vt", [128, 512])
        vext = asb("vext", [128, 4, 132], BF16)
        vw = asb("vw", [128, 4, 132], BF16)
        e1 = asb("e1", [128, 4])
        l1 = asb("l1", [128, 4])
        gg = asb("gg", [128, 4])
        dg = asb("dg", [128, 4, 128])
        Et = asb("Et", [128, 4, 128])
        cmx = asb("cmx", [128, 4])
        Mt = asb("Mt", [128, 4])
        negM = asb("negM", [128, 4])
        bm8 = asb("bm8", [128, 8])
        mend = asb("mend", [128, 8])
        ex12 = asb("ex12", [128, 12])
        E2 = asb("E2", [128, 4, 128])
        DT = asb("DT", [128, 4, 128])
        drow = asb("drow", [128, 4, 128])
        qd = asb("qd", [128, 4, 128], BF16)
        PT = asb("PT", [128, 4, 128], BF16)
        ktok = asb("ktok", [128, 4, 128], BF16)
        rd = asb("rd", [128, 4])
        hd = asb("hd", [128, 512])
        hbt = asb("hbt", [128, 512])
        Cf = asb("Cf", [128, 4, 132])
        Cb = asb("Cb", [128, 4, 132], BF16)
        mcar = [asb("mcar0", [128, 4]), asb("mcar1", [128, 4])]
        psN2 = psN
        sc.op("pool", MK("memset", vext, 1.0), writes=["vext"])
        mstate = {"p": 0, "q": 0}
        ident3 = CF("ident").unsqueeze(1).to_broadcast([128, 4, 128])

        def mstep(b_, i, d):
            gt = b_ * NT + i
            fw = (d == 0)
            io, fo = (0, 4) if fw else (8, 12)
            tri = CF("triF") if fw else CF("triB")
            sel = CF("selF") if fw else CF("selB")
            cmm = (CF("cmF") if fw else CF("cmB")).unsqueeze(1).to_broadcast([128, 4, 128])
            dmm = (CF("dmF") if fw else CF("dmB")).unsqueeze(1).to_broadcast([128, 4, 128])
            mc = mcar[mstate["p"]]
            mcn = mcar[1 - mstate["p"]]
            mct, mcnt = "mcar%d" % mstate["p"], "mcar%d" % (1 - mstate["p"])
            mstate["p"] = 1 - mstate["p"]
            qi = mstate["q"]
            mstate["q"] = 1 - qi
            qk = qkt[qi]
            qtag = "qkt%d" % qi
            sc.dma("sp", qk, qkcd[:, :, gt * 128:(gt + 1) * 128].rearrange("k p t -> p k t"), reads=[("qkcd", b_)], writes=[qtag])
            sc.dma("sp", g16, zt[gt * 128:(gt + 1) * 128, 1280:1296], reads=[("zt", gt)], writes=["g16"])
            sc.dma("sp", vt, zt[gt * 128:(gt + 1) * 128, 256:768], reads=[("zt", gt)], writes=["vt"])
            sc.op("pool", MK("tensor_copy", vext[:, :, 0:128], vt.rearrange("p (j c) -> p j c", j=4)), reads=["vt"], writes=["vext"])
            sc.op("act", MK("activation", e1, g16[:, fo:fo + 4], AF.Exp, scale=-1.0), reads=["g16"], writes=["e1"])
            sc.op("act", MK("activation", l1, e1, AF.Ln, bias=1.0), reads=["e1"], writes=["l1"])
            bs = bankS()
            mm(psS[bs][:, 0:4], tri, l1, True, True, ["cf", "l1"], ["psS%d" % bs])
            sc.op("dve", MK("tensor_tensor", gg, g16[:, io:io + 4], psS[bs][:, 0:4], ALU.add), reads=["g16", "psS%d" % bs], writes=["gg"])
            sc.op("dve", MK("tensor_tensor", dg, ident3, gg.unsqueeze(2).to_broadcast([128, 4, 128]), ALU.mult), reads=["cf", "gg"], writes=["dg"])
            ba = bankA()
            mm(psA[ba][:, :], CF("ones"), dg.rearrange("p j c -> p (j c)"), True, True, ["cf", "dg"], ["psA%d" % ba])
            sc.op("dve", MK("tensor_tensor", Et, psA[ba][:, :].rearrange("p (j c) -> p j c", j=4), cmm, ALU.add), reads=["psA%d" % ba, "cf"], writes=["Et"])
            sc.op("dve", MK("tensor_reduce", cmx, Et, AX.X, ALU.max), reads=["Et"], writes=["cmx"])
            sc.op("dve", MK("tensor_tensor", Mt, cmx, mc, ALU.max), reads=["cmx", mct], writes=["Mt"])
            sc.op("dve", MK("tensor_tensor", bm8[:, 0:4], Mt, psS[bs][:, 0:4], ALU.subtract), reads=["Mt", "psS%d" % bs], writes=["bm8a"])
            sc.op("pool", MK("tensor_copy", bm8[:, 4:8], Mt), reads=["Mt"], writes=["bm8b"])
            bs2 = bankS()
            mm(psS[bs2][:, 0:8], sel, bm8, True, True, ["cf", "bm8a", "bm8b"], ["psS%d" % bs2])
            sc.op("dve", MK("tensor_copy", mend, psS[bs2][:, 0:8]), reads=["psS%d" % bs2], writes=["mend"])
            sc.op("pool", MK("tensor_copy", mcn, mend[:, 0:4]), reads=["mend"], writes=[mcnt])
            sc.op("dve", MK("tensor_tensor", ex12[:, 0:4], gg, mend[:, 4:8], ALU.subtract), reads=["gg", "mend"], writes=["ex12a"])
            sc.op("dve", MK("tensor_tensor", ex12[:, 4:8], mc, mend[:, 4:8], ALU.subtract), reads=[mct, "mend"], writes=["ex12b"])
            sc.op("dve", MK("tensor_tensor", ex12[:, 8:12], psS[bs][:, 0:4], Mt, ALU.subtract), reads=["psS%d" % bs, "Mt"], writes=["ex12c"])
            sc.op("act", MK("activation", ex12, ex12, AF.Exp), reads=["ex12a", "ex12b", "ex12c"], writes=["ex12a", "ex12b", "ex12c", "ex12"])
            sc.op("dve", MK("tensor_scalar", negM, Mt, -1.0, None, ALU.mult), reads=["Mt"], writes=["negM"])
            sc.op("dve", MK("tensor_tensor", dg, ident3, negM.unsqueeze(2).to_broadcast([128, 4, 128]), ALU.mult), reads=["cf", "negM"], writes=["dg"])
            ba2 = bankA()
            mm(psA[ba2][:, :], CF("ones"), dg.rearrange("p j c -> p (j c)"), True, True, ["cf", "dg"], ["psA%d" % ba2])
            sc.op("dve", MK("tensor_tensor", E2, psA[ba2][:, :].rearrange("p (j c) -> p j c", j=4), dmm, ALU.add), reads=["psA%d" % ba2, "cf"], writes=["E2"])
            for j in range(4):
                sc.op("act", MK("activation", DT[:, j, :], E2[:, j, :], AF.Exp, bias=gg[:, j:j + 1]), reads=["E2", "gg"], writes=["DT"])
                sc.op("act", MK("activation", drow[:, j, :], psA[ba2][:, j * 128:(j + 1) * 128], AF.Exp, bias=mc[:, j:j + 1]), reads=["psA%d" % ba2, mct], writes=["drow"])
            sc.op("dve", MK("tensor_tensor", qd, qk[:, 0:4, :], drow, ALU.mult), reads=[qtag, "drow"], writes=["qd"])
            ba3 = bankA()
            for j in range(4):
                mm(psA[ba3][:, j * 128:(j + 1) * 128], qk[:, 4 + j, :], qk[:, j, :], True, True, [qtag], ["psA%d" % ba3])
            sc.op("dve", MK("tensor_tensor", PT.rearrange("p j c -> p (j c)"), psA[ba3][:, :], DT.rearrange("p j c -> p (j c)"), ALU.mult),
                  reads=["psA%d" % ba3, "DT"], writes=["PT"])
            for j in range(4):
                mm(psN[:, j, 0:129], PT[:, j, :], vext[:, j, 0:129], True, False, ["PT", "vext"], ["psN"])
                mm(psN[:, j, 0:129], qd[:, j, :], Cb[:, j, 0:129], False, True, ["qd", "Cb"], ["psN"])
            sc.op("dve", MK("tensor_scalar", rd, psN[:, :, 128], -1.0, None, ALU.mult), reads=["psN"], writes=["rd"])
            sc.op("dve", MK("tensor_tensor", rd, rd, psN[:, :, 128], ALU.max), reads=["psN", "rd"], writes=["rd"])
            sc.op("dve", MK("tensor_tensor", rd, rd, ex12[:, 8:12], ALU.max), reads=["rd", "ex12"], writes=["rd"])
            sc.op("dve", MK("reciprocal", rd, rd), reads=["rd"], writes=["rd"])
            dst = hd if fw else hbt
            dtag = "hd" if fw else "hbt"
            for j in range(4):
                if j % 2:
                    sc.op("act", MK("activation", dst[:, j * 128:(j + 1) * 128], psN[:, j, 0:128], AF.Copy, scale=rd[:, j:j + 1]), reads=["psN", "rd"], writes=[dtag])
                else:
                    sc.op("dve", MK("tensor_scalar", dst[:, j * 128:(j + 1) * 128], psN[:, j, 0:128], rd[:, j:j + 1], None, ALU.mult), reads=["psN", "rd"], writes=[dtag])
            if not fw:
                sc.dma("pool", hb[gt * 128:(gt + 1) * 128, :], hbt, reads=["hbt"], writes=[("hb", gt)])
            sc.op("pool", MK("tensor_tensor", vw, vext, ex12[:, 0:4].unsqueeze(2).to_broadcast([128, 4, 132]), ALU.mult), reads=["vext", "ex12"], writes=["vw"])
            for j in range(4):
                tp(psT[0][:, j * 128:(j + 1) * 128], qk[:, 4 + j, :], CB("ident"), [qtag, "cb"], ["psT0"], sig=(j == 3))
            sc.op("act", MK("copy", ktok.rearrange("p j c -> p (j c)"), psT[0][:, 0:512]), reads=["psT0"], writes=["ktok"])
            for j in range(4):
                mm(psN2[:, j, 0:129], ktok[:, j, :], vw[:, j, 0:129], True, True, ["ktok", "vw"], ["psN"])
            for j in range(4):
                sc.op("dve", MK("scalar_tensor_tensor", Cf[:, j, 0:129], Cf[:, j, 0:129], ex12[:, 4 + j:5 + j], psN2[:, j, 0:129], ALU.mult, ALU.add),
                      reads=["Cf", "ex12", "psN"], writes=["Cf"])
            sc.op("pool", MK("tensor_copy", Cb, Cf), reads=["Cf"], writes=["Cb"])

        pt2 = asb("pt2", [128, 2, 256])
        ptb = asb("ptb", [128, 2, 256], BF16)
        pmb = asb("pmb", [64, 4, 128], BF16)
        poT = asb("poT", [64, 4, 128], BF16)
        uvt = asb("uvt", [128, 512])
        vln = asb("vln", [128, 256])
        vlb = asb("vlb", [128, 256], BF16)
        sgo = asb("sgo", [128, 256], BF16)
        ot = asb("ot", [128, 512])
        hs = asb("hs", [128, 512])
        mlo = asb("mlo", [128, 512], BF16)
        brT = asb("brT", [128, 6, 128], BF16)
        mgt = asb("mgt", [128, D])
        ymid = asb("ymid", [128, D])
        ytmp = asb("ytmp", [128, D])
        ymb = asb("ymb", [128, D], BF16)
        ymT = asb("ymT", [128, 8, 128], BF16)
        x1 = asb("x1", [128, D])
        xn2 = ytmp
        h2f = asb("h2f", [128, 8, 128])
        h2b = asb("h2b", [128, D], BF16)
        oh4 = asb("oh4", [128, 4, E])
        rk = asb("rk", [128, E])
        lg = asb("lg", [128, E])
        mx8 = asb("mx8", [128, 8])
        msk = asb("msk", [128, E])
        cwt = asb("cwt", [128, E])
        cwT = asb("cwT", [E, 128])
        ssum = asb("ssum", [128, 1])

        def fwd_rest(b_, i):
            gt = b_ * NT + i
            isctx = i < 2
            src_i = NB if isctx else b_
            rows = slice(gt * 128, (gt + 1) * 128)
            if isctx:
                g0 = b_ * NT
                sc.dma("sp", pt2[:, 0, :], zt[g0 * 128:(g0 + 1) * 128, 0:256], reads=[("zt", g0)], writes=["pt2"])
                sc.dma("sp", pt2[:, 1, :], zt[(g0 + 1) * 128:(g0 + 2) * 128, 0:256], reads=[("zt", g0 + 1)], writes=["pt2"])
            else:
                sc.dma("sp", pt2[:, 0, :], zt[rows, 0:256], reads=[("zt", gt)], writes=["pt2"])
            sc.op("pool", MK("tensor_copy", ptb, pt2), reads=["pt2"], writes=["ptb"])
            ba = bankA()
            for g in range(4):
                if isctx:
                    for j in range(2):
                        idx = g * 4 + i * 2 + j
                        mm(psA[ba][0:64, g * 128:(g + 1) * 128], ptb[:, j, g * 64:(g + 1) * 64], CB("poolC", idx * 128, 128), j == 0, j == 1, ["ptb", "cb"], ["psA%d" % ba])
                else:
                    mm(psA[ba][0:64, g * 128:(g + 1) * 128], ptb[:, 0, g * 64:(g + 1) * 64], CB("poolL", g * 128, 128), True, True, ["ptb", "cb"], ["psA%d" % ba])
            sc.op("act", MK("copy", pmb.rearrange("p g c -> p (g c)"), psA[ba][0:64, :]), reads=["psA%d" % ba], writes=["pmb"])
            ba = bankA()
            for g in range(4):
                mm(psA[ba][0:64, g * 128:(g + 1) * 128], poolw_b[:, g, :], pmb[:, g, :], True, True, ["poolw_b", "pmb"], ["psA%d" % ba])
            for g in range(4):
                sc.op("act", MK("activation", poT[:, g, :], psA[ba][0:64, g * 128:(g + 1) * 128], AF.Copy, scale=cols_sb[0:64, C_PSC + g:C_PSC + g + 1]),
                      reads=["psA%d" % ba, "cols_sb"], writes=["poT"])
            sc.dma("sp", uvt, zt[rows, 1296:1808], reads=[("zt", gt)], writes=["uvt"])
            layer_norm_stats(uvt[:, 256:512], 256, ["uvt"], st=st2, mv_=mv2, rstd_=rs2, tagp="s2")
            sc.op("dve", MK("tensor_scalar", vln, uvt[:, 256:512], mv2[:, 0:1], rs2[:, 0:1], ALU.subtract, ALU.mult), reads=["uvt", "s2mv", "s2rstd"], writes=["vln"])
            sc.op("pool", MK("tensor_tensor", vln, vln, smallbc[:, 512:768], ALU.mult), reads=["vln", "smallbc"], writes=["vln"])
            sc.op("pool", MK("tensor_tensor", vlb, vln, smallbc[:, 768:1024], ALU.add), reads=["vln", "smallbc"], writes=["vlb"])
            ba = bankA()
            for g in range(4):
                mm(psA[ba][:, g * 64:(g + 1) * 64], sguw_b[:, g, :], vlb[:, g * 64:(g + 1) * 64], True, True, ["sguw_b", "vlb"], ["psA%d" % ba])
            for g in range(4):
                sc.op("dve", MK("scalar_tensor_tensor", sgo[:, g * 64:(g + 1) * 64], psA[ba][:, g * 64:(g + 1) * 64], cols_sb[:, C_BS + g:C_BS + g + 1],
                                                                    uvt[:, g * 64:(g + 1) * 64], ALU.add, ALU.mult),
                      reads=["psA%d" % ba, "cols_sb", "uvt"], writes=["sgo"])
            sc.dma("sp", hbt, hb[rows, :], reads=[("hb", gt)], writes=["hbt"])
            sc.dma("sp", ot, zt[rows, 768:1280], reads=[("zt", gt)], writes=["ot"])
            sc.op("dve", MK("tensor_tensor", hs, hd, hbt, ALU.add), reads=["hd", "hbt"], writes=["hs"])
            for j in range(4):
                layer_norm_stats(hs[:, j * 128:(j + 1) * 128], 128, ["hs"], st=st2, mv_=mv2, rstd_=rs2, eps=HN_EPS, tagp="s2")
                sc.op("dve", MK("tensor_scalar", hs[:, j * 128:(j + 1) * 128], hs[:, j * 128:(j + 1) * 128], mv2[:, 0:1], rs2[:, 0:1], ALU.subtract, ALU.mult),
                      reads=["hs", "s2mv", "s2rstd"], writes=["hs"])
            sc.op("pool", MK("tensor_tensor", hs, hs, smallbc[:, 0:512], ALU.mult), reads=["hs", "smallbc"], writes=["hs"])
            sc.op("pool", MK("tensor_tensor", mlo, hs, ot, ALU.mult), reads=["hs", "ot"], writes=["mlo"])
            for j in range(4):
                tp(psT[0][:, j * 128:(j + 1) * 128], mlo[:, j * 128:(j + 1) * 128], CB("ident"), ["mlo", "cb"], ["psT0"], sig=False)
            for j in range(2):
                tp(psT[0][:, (4 + j) * 128:(5 + j) * 128], sgo[:, j * 128:(j + 1) * 128], CB("ident"), ["sgo", "cb"], ["psT0"], sig=(j == 1))
            sc.op("act", MK("copy", brT.rearrange("p j c -> p (j c)"), psT[0][:, 0:768]), reads=["psT0"], writes=["brT"])
            for bi, (nk, lhs_of, wsb, wtag) in enumerate([(4, lambda g: poT[:, g, :], wbr_p, "wbr_p"), (4, lambda g: brT[:, g, :], wbr_m, "wbr_m"), (2, lambda g: brT[:, 4 + g, :], wbr_s, "wbr_s")]):
                sc.dma("sp", mgt, zt[rows, 1808 + bi * D:1808 + (bi + 1) * D], reads=[("zt", gt)], writes=["mgt"])
                for half in range(2):
                    cs_ = slice(half * 512, (half + 1) * 512)
                    ba = bankA()
                    for g in range(nk):
                        mm(psA[ba][:, :], lhs_of(g), wsb[:, g, cs_], g == 0, g == nk - 1, ["poT", "brT", wtag], ["psA%d" % ba])
                    if bi == 0:
                        sc.op("dve", MK("tensor_tensor", ymid[:, cs_], psA[ba][:, :], mgt[:, cs_], ALU.mult), reads=["psA%d" % ba, "mgt"], writes=["ymid"])
                    else:
                        sc.op("dve", MK("tensor_tensor", ytmp[:, cs_], psA[ba][:, :], mgt[:, cs_], ALU.mult), reads=["psA%d" % ba, "mgt"], writes=["ytmp"])
                        if bi == 1:
                            sc.op("pool", MK("tensor_tensor", ymid[:, cs_], ymid[:, cs_], ytmp[:, cs_], ALU.add), reads=["ymid", "ytmp"], writes=["ymid"])
                        else:
                            sc.op("pool", MK("tensor_tensor", ymb[:, cs_], ymid[:, cs_], ytmp[:, cs_], ALU.add), reads=["ymid", "ytmp"], writes=["ymb"])
            for kc in range(8):
                tp(psT[0][:, kc * 128:(kc + 1) * 128], ymb[:, kc * 128:(kc + 1) * 128], CB("ident"), ["ymb", "cb"], ["psT0"], sig=(kc == 7))
            sc.op("act", MK("copy", ymT.rearrange("p j c -> p (j c)"), psT[0][:, :]), reads=["psT0"], writes=["ymT"])
            for half in range(2):
                for kc in range(8):
                    mm(psF[:, half * 512:(half + 1) * 512], ymT[:, kc, :], wout_sb[:, kc, half * 512:(half + 1) * 512], kc == 0, kc == 7, ["ymT", "wout_sb"], ["psF"])
            k = gt % 2
            sc.dma("sp", xt[k][:], xsrc[rows, :], reads=[("xs", gt)], writes=["xt%d" % k])
            sc.op("dve", MK("tensor_tensor", ytmp, psF[:, :], gbc[:, src_i, :], ALU.mult), reads=["psF", "gbc"], writes=["ytmp"])
            sc.op("dve", MK("scalar_tensor_tensor", x1, xt[k][:], ALPHA, ytmp, ALU.mult, ALU.add), reads=["xt%d" % k, "ytmp"], writes=["x1"])
            layer_norm_stats(x1, D, ["x1"])
            sc.op("dve", MK("tensor_scalar", x1, x1, mv[:, 0:1], rstd[:, 0:1], ALU.subtract, ALU.mult), reads=["x1", "mv", "rstd"], writes=["x1"])
            sc.op("pool", MK("tensor_tensor", x1, x1, lnbc[:, 0, :], ALU.mult), reads=["x1", "lnbc"], writes=["x1"])
            sc.op("pool", MK("tensor_tensor", x1, x1, lnbc[:, 1, :], ALU.add), reads=["x1", "lnbc"], writes=["x1"])
            sc.dma("pool", xs[rows, :], x1, reads=["x1"], writes=[("xs", gt)])
            layer_norm_stats(x1, D, ["x1"])
            sc.op("dve", MK("tensor_scalar", xn2, x1, mv[:, 0:1], rstd[:, 0:1], ALU.subtract, ALU.mult), reads=["x1", "mv", "rstd", "ytmp"], writes=["ytmp"])
            for half in range(2):
                ba = bankA()
                for kk in range(4):
                    kc = half * 4 + kk
                    tp(psA[ba][:, kk * 128:(kk + 1) * 128], xn2[:, kc * 128:(kc + 1) * 128], CF("ident"), ["ytmp", "cf"], ["psA%d" % ba], sig=(kk == 3))
                for kk in range(4):
                    kc = half * 4 + kk
                    sc.op("act", MK("activation", h2f[:, kc, :], psA[ba][:, kk * 128:(kk + 1) * 128], AF.Identity,
                                                                     bias=modc[:, 16 + kc, src_i:src_i + 1], scale=modc[:, 24 + kc, src_i:src_i + 1]),
                          reads=["psA%d" % ba, "modc"], writes=["h2f"])
            for half in range(2):
                ba = bankA()
                for kk in range(4):
                    kc = half * 4 + kk
                    tp(psA[ba][:, kk * 128:(kk + 1) * 128], h2f[:, kc, :], CF("ident"), ["h2f", "cf"], ["psA%d" % ba], sig=(kk == 3))
                sc.op("act" if half else "dve", (MK("copy", h2b[:, half * 512:(half + 1) * 512], psA[ba][:, :])) if half else
                      (MK("tensor_copy", h2b[:, half * 512:(half + 1) * 512], psA[ba][:, :])), reads=["psA%d" % ba], writes=["h2b"])
            sc.dma("pool", h2d[rows, :], h2b, reads=["h2b"], writes=[("h2d", gt)])
            bs = bankS()
            for kc in range(8):
                mm(psS[bs][:, 0:E], h2f[:, kc, :], wr_sb[:, kc, :], kc == 0, False, ["h2f", "wr_sb"], ["psS%d" % bs])
            mm(psS[bs][:, 0:E], CF("ones", 0, 128)[0:1, :], brow[0:1, 0:E], False, True, ["cf", "brow"], ["psS%d" % bs])
            sc.op("dve", MK("tensor_copy", lg, psS[bs][:, 0:E]), reads=["psS%d" % bs], writes=["lg"])
            sc.op("dve", MK("max", out=mx8, in_=lg), reads=["lg"], writes=["mx8"])
            sc.op("dve", MK("tensor_scalar", msk, lg, mx8[:, 3:4], None, ALU.is_ge), reads=["lg", "mx8"], writes=["msk"])
            for kq in range(4):
                sc.op("dve", MK("tensor_scalar", oh4[:, kq, :], lg, mx8[:, kq:kq + 1], None, ALU.is_ge), reads=["lg", "mx8"], writes=["oh4"])
            for kq in range(3, 0, -1):
                sc.op("dve", MK("tensor_tensor", oh4[:, kq, :], oh4[:, kq, :], oh4[:, kq - 1, :], ALU.subtract), reads=["oh4"], writes=["oh4"])
            sc.dma("pool", ohd[rows, :], oh4.rearrange("p k e -> p (k e)"), reads=["oh4"], writes=[("ohd", gt)])
            bs3 = bankS()
            mm(psS[bs3][:, 0:E], CF("triS"), msk, True, True, ["cf", "msk"], ["psS%d" % bs3])
            sc.op("dve", MK("tensor_tensor", rk, psS[bs3][:, 0:E], carry[:], ALU.add), reads=["psS%d" % bs3, "carry"], writes=["rk"])
            sc.dma("pool", rankd[rows, :], rk, reads=["rk"], writes=[("rankd", gt)])
            bs4 = bankS()
            mm(psS[bs4][:, 0:E], CF("ones"), msk, True, True, ["cf", "msk"], ["psS%d" % bs4])
            sc.op("dve", MK("tensor_tensor", carry[:], carry[:], psS[bs4][:, 0:E], ALU.add), reads=["psS%d" % bs4, "carry"], writes=["carry"])
            sc.op("dve", MK("tensor_scalar", lg, lg, mx8[:, 0:1], None, ALU.subtract), reads=["lg", "mx8"], writes=["lg"])
            sc.op("act", MK("activation", lg, lg, AF.Exp), reads=["lg"], writes=["lg"])
            sc.op("dve", MK("tensor_tensor", lg, lg, msk, ALU.mult), reads=["lg", "msk"], writes=["lg"])
            sc.op("dve", MK("tensor_reduce", ssum, lg, AX.X, ALU.add), reads=["lg"], writes=["ssum"])
            sc.op("dve", MK("reciprocal", ssum, ssum), reads=["ssum"], writes=["ssum"])
            sc.op("dve", MK("tensor_scalar", cwt, lg, ssum[:, 0:1], None, ALU.mult), reads=["lg", "ssum"], writes=["cwt"])
            sc.dma("pool", cwd[rows, :], cwt, reads=["cwt"], writes=[("cw", gt)])

        for b_ in range(NB):
            base = b_ * LT
            for cc in range(8):
                w0 = cols_sb[:, C_CW + cc:C_CW + cc + 1]
                w1 = cols_sb[:, C_CW + 8 + cc:C_CW + 8 + cc + 1]
                w2 = cols_sb[:, C_CW + 16 + cc:C_CW + 16 + cc + 1]
                bb = cols_sb[:, C_CB + cc:C_CB + cc + 1]
                for (s0_, s1_) in [(0, CTX), (CTX, LT)]:
                    for a in range(s0_, s1_, CP):
                        bnd = min(s1_, a + CP)
                        n = bnd - a
                        lo, hi = max(a - 1, s0_), min(bnd + 1, s1_)
                        off = lo - (a - 1)
                        sc.op("pool", MK("memset", craw, 0.0), writes=["craw"])
                        sc.dma("sp", craw[:, off:off + hi - lo], qkT[cc, :, base + lo:base + hi], reads=[("qkT", b_ * NT + i) for i in range(NT)], writes=["craw"])
                        sc.op("dve", MK("tensor_scalar", ctmp[:, 0:n], craw[:, 1:n + 1], w1, bb, ALU.mult, ALU.add), reads=["craw", "cols_sb"], writes=["ctmp"])
                        sc.op("dve", MK("scalar_tensor_tensor", ctmp[:, 0:n], craw[:, 0:n], w0, ctmp[:, 0:n], ALU.mult, ALU.add), reads=["craw", "ctmp", "cols_sb"], writes=["ctmp"])
                        sc.op("dve", MK("scalar_tensor_tensor", ctmp[:, 0:n], craw[:, 2:n + 2], w2, ctmp[:, 0:n], ALU.mult, ALU.add), reads=["craw", "ctmp", "cols_sb"], writes=["ctmp"])
                        if cc < 4:
                            sc.op("act", MK("activation", cob[:, 0:n], ctmp[:, 0:n], AF.Silu), reads=["ctmp"], writes=["cob"])
                        else:
                            sc.op("act", MK("activation", ctmp[:, 0:n], ctmp[:, 0:n], AF.Silu), reads=["ctmp"], writes=["ctmp"])
                            sc.op("dve", MK("tensor_scalar", cob[:, 0:n], ctmp[:, 0:n], 128.0 ** -0.5, None, ALU.mult), reads=["ctmp"], writes=["cob"])
                        sc.dma("pool", qkcd[cc, :, base + a:base + bnd], cob[:, 0:n], reads=["cob"], writes=[("qkcd", b_)])
            for d, order in [(1, [1, 0] + list(range(NT - 1, 1, -1))), (0, list(range(NT)))]:
                sc.op("pool", MK("memset", Cf, 0.0), writes=["Cf"])
                sc.op("pool", MK("memset", Cb, 0.0), writes=["Cb"])
                sc.op("pool", MK("memset", mcar[mstate["p"]], 0.0), writes=["mcar%d" % mstate["p"]])
                for i in order:
                    mstep(b_, i, d)
                    if d == 0 and not (last and i < 2):
                        fwd_rest(b_, i)

        new_phase()
        IOA = bass.IndirectOffsetOnAxis
        MAGIC = 12582912.0
        nE = asb("nE", [128, E])
        padE = asb("padE", [128, E])
        incE = asb("incE", [128, E])
        offE = asb("offE", [128, E])
        onesE = asb("onesE", [128, E])
        cmp3 = asb("cmp3", [128, NTILE, E])
        teb = asb("teb", [128, NTILE])
        idxg_f = asb("idxg_f", [128, NTILE, 8])
        idxd_f = asb("idxd_f", [128, NTILE])
        rkt = asb("rkt", [128, E])
        oht = asb("oht", [128, 4, E])
        cwl = asb("cwl", [128, E])
        prod = asb("prod", [128, 4, E])
        pos4f = asb("pos4f", [128, 4])
        gb_ = [asb("gb%d" % i, [128, TS]) for i in range(2)]
        sb_ = [asb("sgm%d" % i, [128, TS]) for i in range(2)]
        ub_ = [asb("ub%d" % i, [128, TS]) for i in range(2)]
        bguj = [asb("bguj%d" % i, [128, 16]) for i in range(2)]
        ysb = [asb("ysb%d" % i, [128, D]) for i in range(2)]
        yk = [asb("yk%d" % i, [128, D]) for i in range(2)]
        accm = asb("accm", [128, D])
        x1 = asb("x1m", [128, D])
        bdn_sb = asb("bdn_sb", [E, D])
        cwTt = asb("cwTt", [E, 128])
        hsl = asb("hsl", [128, 4, D], BF16)
        hgT = asb("hgT", [128, 8, TS], BF16)
        wgj = [asb("wgj%d" % i, [128, 8, 256], BF16) for i in range(2)]
        wd = [asb("wd%d" % i, [128, 8, D], BF16) for i in range(2)]
        actb = asb("actb", [128, 8, TS], BF16)
        sc.dma("sp", bdn_sb, bdn_in[l], writes=["bdn_sb"])
        sc.op("pool", MK("memset", onesE, 1.0), writes=["onesE"])
        sc.op("dve", MK("tensor_scalar", nE, carry[:], float(TS - 1), 1.0 / TS, ALU.add, ALU.mult), reads=["carry"], writes=["nE"])
        sc.op("dve", MK("tensor_scalar", nE, nE, -0.5 + 1.0 / 1024, MAGIC, ALU.add, ALU.add), reads=["nE"], writes=["nE"])
        sc.op("dve", MK("tensor_scalar", padE, nE, -MAGIC, float(TS), ALU.add, ALU.mult), reads=["nE"], writes=["padE"])
        sc.op("dve", MK("tensor_tensor_scan", incE, onesE, padE, 0.0, ALU.mult, ALU.add), reads=["onesE", "padE"], writes=["incE"])
        sc.op("dve", MK("tensor_tensor", offE, incE, padE, ALU.subtract), reads=["incE", "padE"], writes=["offE"])
        sc.op("dve", MK("tensor_tensor", cmp3, CF("jts", 0, NTILE).unsqueeze(2).to_broadcast([128, NTILE, E]), incE.unsqueeze(1).to_broadcast([128, NTILE, E]), ALU.is_ge),
              reads=["incE", "cf"], writes=["cmp3"])
        sc.op("dve", MK("tensor_reduce", teb, cmp3, AX.X, ALU.add), reads=["cmp3"], writes=["teb"])
        sc.op("dve", MK("tensor_scalar", teb, teb, float(E - 1), None, ALU.min), reads=["teb"], writes=["teb"])
        sc.op("dve", MK("tensor_scalar", idxg_f, teb.unsqueeze(2).to_broadcast([128, NTILE, 8]), 1024.0, None, ALU.mult), reads=["teb"], writes=["idxg_f"])
        sc.op("dve", MK("tensor_tensor", idxg_f, idxg_f, CF("cg", 0, 8).unsqueeze(1).to_broadcast([128, NTILE, 8]), ALU.add), reads=["idxg_f", "cf"], writes=["idxg_f"])
        sc.op("dve", MK("tensor_copy", idxg_u[:], idxg_f.rearrange("p j k -> p (j k)")), reads=["idxg_f"], writes=["idxg_u"])
        sc.op("dve", MK("tensor_scalar", idxd_f, teb, 128.0, CF("cg", 0, 1), ALU.mult, ALU.add), reads=["teb", "cf"], writes=["idxd_f"])
        sc.op("dve", MK("tensor_copy", idxd_u[:], idxd_f), reads=["idxd_f"], writes=["idxd_u"])

        def routed(gt):
            b_, i = gt // NT, gt % NT
            return not (last and i < 2)

        for gt in range(NTT):
            if not routed(gt):
                continue
            rows = slice(gt * 128, (gt + 1) * 128)
            sc.dma("sp", rkt, rankd[rows, :], reads=[("rankd", gt)], writes=["rkt"])
            sc.dma("sp", oht, ohd[rows, :].rearrange("p (k e) -> p k e", k=4), reads=[("ohd", gt)], writes=["oht"])
            sc.dma("sp", cwl, cwd[rows, :], reads=[("cw", gt)], writes=["cwl"])
            sc.dma("sp", hsl[:, 0, :], h2d[rows, :], reads=[("h2d", gt)], writes=["hsl"])
            sc.op("dve", MK("tensor_tensor", rkt, rkt, offE, ALU.add), reads=["rkt", "offE"], writes=["rkt"])
            sc.op("dve", MK("tensor_tensor", prod, oht, rkt.unsqueeze(1).to_broadcast([128, 4, E]), ALU.mult), reads=["oht", "rkt"], writes=["prod"])
            sc.op("dve", MK("tensor_reduce", pos4f, prod, AX.X, ALU.add), reads=["prod"], writes=["pos4f"])
            sc.op("dve", MK("tensor_copy", pos4_all[:, gt * 4:gt * 4 + 4], pos4f), reads=["pos4f"], writes=[("pos4", gt)])
            sc.op("dve", MK("tensor_tensor", prod, oht, cwl.unsqueeze(1).to_broadcast([128, 4, E]), ALU.mult), reads=["oht", "cwl", "pos4f"], writes=["prod"])
            sc.op("dve", MK("tensor_reduce", cw4_all[:, gt * 4:gt * 4 + 4], prod, AX.X, ALU.add), reads=["prod"], writes=[("cw4", gt)])
            for kq in range(4):
                sc.dma("pool", Hs[:, :], hsl[:, 0, :], reads=["hsl", ("pos4", gt)], writes=["Hs"], method="indirect_dma_start",
                       out_offset=IOA(pos4_all[:, gt * 4 + kq:gt * 4 + kq + 1], 0), in_offset=None)
        for jt in range(NTILE):
            kb = jt % 2
            sc.dma("pool", bguj[kb], bgu_in[l], reads=["idxd_u"], writes=["bguj%d" % kb], method="indirect_dma_start",
                   out_offset=None, in_offset=IOA(idxd_u[:, jt:jt + 1], 0))
            kd = jt % 2
            sc.dma("pool", wd[kd].rearrange("p f d -> p (f d)"), wb_dn[l], reads=["idxd_u", WL("dn")], writes=["wd%d" % kd], method="indirect_dma_start",
                   out_offset=None, in_offset=IOA(idxd_u[:, jt:jt + 1], 0))
            sc.dma("sp", hsl, Hs[jt * TS:(jt + 1) * TS, :].rearrange("(a p) d -> p a d", p=128), reads=["Hs"], writes=["hsl"])
            for a in range(4):
                for kc in range(8):
                    tp(psT[0][:, kc * 128:(kc + 1) * 128], hsl[:, a, kc * 128:(kc + 1) * 128], CB("ident"), ["hsl", "cb"], ["psT0"], sig=(kc == 7))
                sc.op("act" if a % 2 else "dve", (MK("copy", hgT[:, :, a * 128:(a + 1) * 128], psT[0][:, :].rearrange("p (k t) -> p k t", k=8))) if a % 2 else
                      (MK("tensor_copy", hgT[:, :, a * 128:(a + 1) * 128], psT[0][:, :].rearrange("p (k t) -> p k t", k=8))), reads=["psT0"], writes=["hgT"])
            for j in range(8):
                kg = (jt * 8 + j) % 2
                sc.dma("pool", wgj[kg].rearrange("p k c -> p (k c)"), wb_gu[l], reads=["idxg_u", WL("gu")], writes=["wgj%d" % kg], method="indirect_dma_start",
                       out_offset=None, in_offset=IOA(idxg_u[:, jt * 8 + j:jt * 8 + j + 1], 0))
                for kc in range(8):
                    mm(psA[0][:, 0:TS], wgj[kg][:, kc, 0:128], hgT[:, kc, :], kc == 0, kc == 7, ["wgj%d" % kg, "hgT"], ["psA0"])
                for kc in range(8):
                    mm(psA[1][:, 0:TS], wgj[kg][:, kc, 128:256], hgT[:, kc, :], kc == 0, kc == 7, ["wgj%d" % kg, "hgT"], ["psA1"])
                q_ = j % 2
                sc.op("dve", MK("tensor_scalar", gb_[q_], psA[0][:, 0:TS], bguj[kb][:, j:j + 1], 7.0, ALU.add, ALU.min), reads=["psA0", "bguj%d" % kb], writes=["gb%d" % q_])
                sc.op("act", MK("activation", sb_[q_], gb_[q_], AF.Sigmoid, scale=1.702), reads=["gb%d" % q_], writes=["sgm%d" % q_])
                sc.op("pool", MK("tensor_tensor", gb_[q_], gb_[q_], sb_[q_], ALU.mult), reads=["gb%d" % q_, "sgm%d" % q_], writes=["gb%d" % q_])
                sc.op("dve", MK("tensor_scalar", ub_[q_], psA[1][:, 0:TS], bguj[kb][:, 8 + j:9 + j], 7.0, ALU.add, ALU.min), reads=["psA1", "bguj%d" % kb], writes=["ub%d" % q_])
                sc.op("pool", MK("tensor_scalar", ub_[q_], ub_[q_], -7.0, 1.0, ALU.max, ALU.add), reads=["ub%d" % q_], writes=["ub%d" % q_])
                sc.op("dve", MK("tensor_tensor", actb[:, j, :], ub_[q_], gb_[q_], ALU.mult), reads=["ub%d" % q_, "gb%d" % q_], writes=[("actb", j)])
            for tt in range(4):
                ky = tt % 2
                for half in range(2):
                    for f in range(8):
                        mm(psF[:, half * 512:(half + 1) * 512], actb[:, f, tt * 128:(tt + 1) * 128], wd[kd][:, f, half * 512:(half + 1) * 512], f == 0, f == 7,
                           [("actb", f_) for f_ in range(8)] + ["wd%d" % kd], ["psF%d" % half])
                    if half:
                        sc.op("act", MK("copy", ysb[ky][:, 512:1024], psF[:, 512:1024]), reads=["psF1"], writes=["ysb%d" % ky])
                    else:
                        sc.op("dve", MK("tensor_copy", ysb[ky][:, 0:512], psF[:, 0:512]), reads=["psF0"], writes=["ysb%d" % ky])
                r0 = jt * TS + tt * 128
                sc.dma("sp", Yd[r0:r0 + 128, :], ysb[ky], reads=["ysb%d" % ky], writes=[("Yd", jt, tt)])
        join([("Yd", jt, tt) for jt in range(NTILE) for tt in range(4)], "YdAll")
        for gt in range(NTT):
            if not routed(gt):
                continue
            b_, i = gt // NT, gt % NT
            rows = slice(gt * 128, (gt + 1) * 128)
            src_i = NB if i < 2 else b_
            k = gt % 2
            sc.dma("sp", xt[k][:], xs[rows, :], reads=[("xs", gt)], writes=["xt%d" % k])
            sc.dma("sp", cwl, cwd[rows, :], reads=[("cw", gt)], writes=["cwl"])
            bs = bankS()
            tp(psS[bs][0:E, 0:128], cwl, CF("ident"), ["cwl", "cf"], ["psS%d" % bs])
            sc.op("act", MK("copy", cwTt, psS[bs][0:E, 0:128]), reads=["psS%d" % bs], writes=["cwTt"])
            for half in range(2):
                mm(psF[:, half * 512:(half + 1) * 512], cwTt, bdn_sb[:, half * 512:(half + 1) * 512], True, True, ["cwTt", "bdn_sb"], ["psF%d" % half])
            sc.op("act", MK("copy", accm, psF[:, :]), reads=["psF0", "psF1"], writes=["accm"])
            for kq in range(4):
                ky = kq % 2
                sc.dma("pool", yk[ky], Yd[:, :], reads=["YdAll", ("pos4", gt)], writes=["yk%d" % ky], method="indirect_dma_start",
                       out_offset=None, in_offset=IOA(pos4_all[:, gt * 4 + kq:gt * 4 + kq + 1], 0))
                sc.op("dve", MK("scalar_tensor_tensor", accm, yk[ky], cw4_all[:, gt * 4 + kq:gt * 4 + kq + 1], accm, ALU.mult, ALU.add),
                      reads=["yk%d" % ky, ("cw4", gt), "accm"], writes=["accm"])
            sc.op("pool", MK("tensor_tensor", accm, accm, gbc[:, (NB + 1) + src_i, :], ALU.mult), reads=["accm", "gbc"], writes=["accm"])
            sc.op("dve", MK("scalar_tensor_tensor", x1, xt[k][:], ALPHA, accm, ALU.mult, ALU.add), reads=["xt%d" % k, "accm"], writes=["x1m"])
            layer_norm_stats(x1, D, ["x1m"])
            sc.op("dve", MK("tensor_scalar", x1, x1, mv[:, 0:1], rstd[:, 0:1], ALU.subtract, ALU.mult), reads=["x1m", "mv", "rstd"], writes=["x1m"])
            sc.op("pool", MK("tensor_tensor", x1, x1, lnbc[:, 2, :], ALU.mult), reads=["x1m", "lnbc"], writes=["x1m"])
            sc.op("pool", MK("tensor_tensor", x1, x1, lnbc[:, 3, :], ALU.add), reads=["x1m", "lnbc"], writes=["x1m"])
            if last:
                orow = b_ * S + (i - 2) * 128
                sc.dma("pool", out[orow:orow + 128, :], x1, reads=["x1m"], writes=[("out", gt)])
            else:
                sc.dma("pool", xs[rows, :], x1, reads=["x1m"], writes=[("xs", gt)])

    evs = [sc.lastw[t] for t in list(sc.lastw) if isinstance(t, tuple) and t[0] == "out"]
    sc.final_wait("pool", evs)
    sc.emit()
    return nc, stack


def _prep_inputs(inp, cfg):
    NB, S, DEPTH, E, NCORES = cfg["NB"], cfg["S"], cfg["DEPTH"], cfg["E"], cfg["NCORES"]
    f = lambda a: np.asarray(a, dtype=np.float32)
    L = DEPTH
    consts = make_consts()
    cmat = np.ascontiguousarray(np.concatenate([consts[k] for k in CONST_ORDER], axis=1).astype(np.float32))
    rows = np.zeros((L, 1, 17408), np.float32)
    rows[:, 0, 0:6144] = f(inp["b_ada"])
    rows[:, 0, 6144:6144 + IN_COLS] = f(inp["b_in"])
    rows[:, 0, 12048:13072] = f(inp["ln1_g"])
    rows[:, 0, 13072:14096] = f(inp["ln1_b"])
    rows[:, 0, 14096:15120] = f(inp["ln2_g"])
    rows[:, 0, 15120:16144] = f(inp["ln2_b"])
    rows[:, 0, 16144:16656] = f(inp["mlstm_norm_g"])
    rows[:, 0, 16656:16912] = f(inp["sgu_ln_g"])
    rows[:, 0, 16912:17168] = f(inp["sgu_ln_b"])
    rows[:, 0, 17168:17168 + E] = f(inp["b_router"])
    cols = np.zeros((L, 128, 256), np.float32)
    cols[:, :, 0:48] = f(inp["b_ada"]).reshape(L, 48, 128).transpose(0, 2, 1)
    cols[:, :, 48:56] = f(inp["b_in"])[:, 256:1280].reshape(L, 8, 128).transpose(0, 2, 1)
    cols[:, :, 56:80] = f(inp["qk_conv_w"]).reshape(L, 3, 8, 128).transpose(0, 3, 1, 2).reshape(L, 128, 24)
    cols[:, :, 80:88] = f(inp["qk_conv_b"]).reshape(L, 8, 128).transpose(0, 2, 1)
    cols[:, 0:64, 88:92] = f(inp["pool_scale"]).reshape(L, 4, 64).transpose(0, 2, 1)
    cols[:, :, 92:96] = f(inp["sgu_b"]).transpose(0, 2, 1)
    pool_w = np.ascontiguousarray(f(inp["pool_w"]).transpose(0, 2, 1, 3))
    sgu_wT = np.ascontiguousarray(f(inp["sgu_w"]).transpose(0, 3, 1, 2))
    bgu = np.ascontiguousarray(f(inp["b_gate_up"]).reshape(L, E, 16, 128).transpose(0, 1, 3, 2).reshape(L, E * 128, 16))
    bdn = np.ascontiguousarray(f(inp["b_down"]))
    w_router = np.ascontiguousarray(f(inp["w_router"]).reshape(L, 8, 128, E).transpose(0, 2, 1, 3))
    w_br = np.concatenate([f(inp["w_br_pool"]), f(inp["w_br_mlstm"]), f(inp["w_br_sgu"])], axis=1)
    w_gu = f(inp["w_gate_up"]).reshape(L, E, 8, 128, 2, 8, 128).transpose(0, 1, 5, 3, 2, 4, 6).reshape(L, E * D, 2 * D)
    w_dn = f(inp["w_down"]).reshape(L, E, 8, 128, D).transpose(0, 1, 3, 2, 4).reshape(L, E * 128, 8 * D)
    big = {"w_ada": f(inp["w_ada"]), "w_in": f(inp["w_in"]), "w_br": w_br, "w_out": f(inp["w_out"]), "w_gu": w_gu, "w_dn": w_dn}
    x, ctx, c, c_ctx = f(inp["x"]), f(inp["ctx"]), f(inp["c"]), f(inp["c_ctx"])
    maps = []
    for r in range(NCORES):
        m = {}
        bsl = range(r * NB, (r + 1) * NB)
        m["xin"] = np.ascontiguousarray(np.concatenate([np.concatenate([ctx[b], x[b]], axis=0) for b in bsl], axis=0))
        cs = np.stack([c[b] for b in bsl] + [c_ctx], axis=1)
        m["cT"] = np.ascontiguousarray(cs.reshape(8, 128, NB + 1).transpose(1, 0, 2))
        m["consts"] = cmat
        oh = np.zeros((128, 8), np.float32)
        oh[:, r] = 1.0
        m["onehot"] = oh
        for k, w in big.items():
            rs = w.shape[1] // NCORES
            m[k] = np.ascontiguousarray(w[:, r * rs:(r + 1) * rs, :])
        m["w_router"] = w_router
        m["rows"] = rows
        m["cols"] = cols
        m["pool_w"] = pool_w
        m["sgu_wT"] = sgu_wT
        for l_ in range(L):
            m["bgu%d" % l_] = np.ascontiguousarray(bgu[l_])
        m["bdn"] = bdn
        maps.append(m)
    return maps


def run(inp, cfg):
    NB, S, NCORES = cfg["NB"], cfg["S"], cfg["NCORES"]
    nc, stack = build(cfg)
    maps = _prep_inputs(inp, cfg)
    res = run_bass_kernel_spmd(nc, maps, core_ids=list(range(NCORES)))
    outs = [np.asarray(res.results[r]["out"]).reshape(NB, S, D) for r in range(NCORES)]
    return np.concatenate(outs, axis=0).astype(np.float32)


def kernel(**inp):
    cfg = dict(NB=2, S=4096, DEPTH=4, E=32, NCORES=8)
    return run(inp, cfg)
```

```python
import math
from contextlib import ExitStack
import numpy as np
import ml_dtypes
import concourse.bass as bass
import concourse.mybir as mybir
from concourse.bass_utils import run_bass_kernel_spmd

F32 = mybir.dt.float32
BF16 = mybir.dt.bfloat16
ALU = mybir.AluOpType
AF = mybir.ActivationFunctionType
AX = mybir.AxisListType

D = 1024
CTX = 256
GRID_W = 64
POOL_W = (2, 4, 8, 16)
IN_COLS = 5904
NTM = 4880
LN_EPS = 1e-5
HN_EPS = 1e-6
ALPHA = 8.0 ** 0.25
SEM_LIMIT = 30000
NEG = -1.0e30


class Sched:
    def __init__(self, nc, stack):
        self.nc = nc
        self.stack = stack
        self.engs = {"pe": nc.tensor, "dve": nc.vector, "act": nc.scalar, "pool": nc.gpsimd, "sp": nc.sync}
        self.ops = {k: [] for k in self.engs}
        self.sems = []
        self.cur = {}
        self.covered = {}
        self.lastw = {}
        self.reads = {}
        self.dma_n = {}
        self.dma_slots = {}
        self.NDS = 8
        self.nsem = 0

    def new_sem(self, owner=None):
        s = self.stack.enter_context(self.nc.semaphore("s%d" % self.nsem))
        self.nsem += 1
        self.sems.append(s)
        self.owner = getattr(self, "owner", {})
        self.owner[len(self.sems) - 1] = owner
        return len(self.sems) - 1

    def _deps(self, eng, reads, writes):
        deps = set()
        for t in list(reads) + list(writes):
            e = self.lastw.get(t)
            if e is not None:
                deps.add(e)
        for t in writes:
            for e in self.reads.get(t, ()):
                deps.add(e)
        waits = []
        for (si, v) in sorted(deps):
            if eng == "pe" and self.owner.get(si) == "pe":
                continue
            if self.covered.get((eng, si), 0) >= v:
                continue
            self.covered[(eng, si)] = v
            waits.append((si, v))
        return waits

    def _record(self, ev, reads, writes):
        for t in writes:
            self.lastw[t] = ev
            self.reads[t] = []
        for t in reads:
            self.reads.setdefault(t, []).append(ev)

    def op(self, eng, fn, reads=(), writes=(), sig=True):
        waits = self._deps(eng, reads, writes)
        if eng not in self.cur:
            self.cur[eng] = [self.new_sem(eng), 0]
        c = self.cur[eng]
        ev = (c[0], c[1] + 1)
        if sig:
            c[1] += 1
        self.ops[eng].append((waits, fn, (c[0], 1) if sig else None))
        self._record(ev, reads, writes)
        if sig and c[1] >= SEM_LIMIT:
            self.cur[eng] = [self.new_sem(eng), 0]
        return ev

    def dma(self, q, out, in_, reads=(), writes=(), method="dma_start", **kw):
        n = self.dma_n.get(q, 0)
        self.dma_n[q] = n + 1
        slot = n % self.NDS
        key = (q, slot)
        st = self.dma_slots.get(key)
        if st is None or st[1] + 16 > SEM_LIMIT:
            prev = st
            st = [self.new_sem("dma"), 0]
            self.dma_slots[key] = st
            waits0 = [(prev[0], prev[1])] if prev is not None else []
        else:
            waits0 = [(st[0], st[1])] if st[1] > 0 else []
        waits = self._deps(q, reads, writes)
        for (si, v) in waits0:
            if self.covered.get((q, si), 0) < v:
                self.covered[(q, si)] = v
                waits.append((si, v))
        st[1] += 16
        ev = (st[0], st[1])
        self.ops[q].append((waits, (method, (), dict(out=out, in_=in_, **kw)), (st[0], 16)))
        self._record(ev, reads, writes)
        return ev

    def barrier(self):
        evs = []
        for eng, c in self.cur.items():
            if c[1] > 0:
                evs.append((c[0], c[1]))
            elif c[0] > 0 and self.owner.get(c[0] - 0) == eng:
                for si in range(c[0] - 1, -1, -1):
                    if self.owner.get(si) == eng:
                        evs.append((si, SEM_LIMIT))
                        break
        for key, st in self.dma_slots.items():
            if st[1] > 0:
                evs.append((st[0], st[1]))
        for eng in self.engs:
            w = []
            for (si, v) in evs:
                if self.covered.get((eng, si), 0) < v:
                    self.covered[(eng, si)] = v
                    w.append((si, v))
            if w:
                self.ops[eng].append((w, None, None))

    def final_wait(self, eng, evs):
        self.ops[eng].append(([(si, v) for (si, v) in evs], None, None))

    def emit(self):
        nc = self.nc
        sems = self.sems
        with nc.Block() as block:
            def mk(name):
                def body(e):
                    for (waits, fn, inc) in self.ops[name]:
                        for (si, v) in waits:
                            e.wait_ge(sems[si], v)
                        if fn is None:
                            continue
                        ins = getattr(e, fn[0])(*fn[1], **fn[2])
                        if inc is not None:
                            ins.then_inc(sems[inc[0]], inc[1])
                return body
            block.tensor(mk("pe"))
            block.vector(mk("dve"))
            block.scalar(mk("act"))
            block.gpsimd(mk("pool"))
            block.sync(mk("sp"))


def MK(m, *a, **k):
    return (m, a, k)


class PerLayer:
    def __init__(self, ts):
        self.ts = ts

    def __getitem__(self, key):
        if isinstance(key, tuple):
            return self.ts[key[0]][key[1:]]
        return self.ts[key]


def make_consts():
    I = np.eye(128, dtype=np.float32)
    s = np.arange(128)[:, None]
    t = np.arange(128)[None, :]
    c = {}
    c["ident"] = I
    c["ones"] = np.ones((128, 128), np.float32)
    c["triF"] = (s <= t).astype(np.float32)
    c["triB"] = (s >= t).astype(np.float32)
    c["triS"] = (s < t).astype(np.float32)
    c["jts"] = np.repeat((np.arange(128, dtype=np.float32) * 512.0)[None, :], 128, axis=0)
    cg = np.zeros((128, 128), np.float32)
    cg[:, 0:8] = np.arange(8)[None, :] * 128.0 + np.arange(128)[:, None]
    c["cg"] = cg
    c["selF"] = np.repeat((s == 127).astype(np.float32), 128, axis=1)
    c["selB"] = np.repeat((s == 0).astype(np.float32), 128, axis=1)
    mF = np.where(t <= s, 0.0, NEG).astype(np.float32)
    mB = np.where(t >= s, 0.0, NEG).astype(np.float32)
    c["cmF"] = mF
    c["cmB"] = mB
    dF = np.where(s <= t, 0.0, NEG).astype(np.float32)
    dB = np.where(s >= t, 0.0, NEG).astype(np.float32)
    c["dmF"] = dF
    c["dmB"] = dB

    def band(length, w):
        tt = np.arange(length)
        lo = np.clip(tt - w // 2, 0, length)
        hi = np.clip(tt + w // 2, 0, length)
        P = np.zeros((length, length), np.float32)
        for a in range(length):
            P[a, lo[a]:hi[a]] = 1.0 / (hi[a] - lo[a])
        return P - np.eye(length, dtype=np.float32)
    pl = []
    for w in POOL_W:
        P64 = band(64, w)
        P = np.zeros((128, 128), np.float32)
        P[:64, :64] = P64
        P[64:, 64:] = P64
        pl.append(P.T.copy())
    c["poolL"] = np.concatenate(pl, axis=1)
    pc = []
    for w in POOL_W:
        P = band(256, w).T
        for i in range(2):
            for j in range(2):
                pc.append(P[j * 128:(j + 1) * 128, i * 128:(i + 1) * 128].copy())
    c["poolC"] = np.concatenate(pc, axis=1)
    return c


CONST_ORDER = ["ident", "ones", "triF", "triB", "selF", "selB", "cmF", "cmB", "dmF", "dmB", "triS", "jts", "cg", "poolL", "poolC"]


def const_offsets():
    c = make_consts()
    off = {}
    o = 0
    for k in CONST_ORDER:
        off[k] = (o, c[k].shape[1])
        o += c[k].shape[1]
    return off, o


def build(cfg):
    NB, S, DEPTH, E, NCORES = cfg["NB"], cfg["S"], cfg["DEPTH"], cfg["E"], cfg["NCORES"]
    LT = CTX + S
    NT = LT // 128
    NTOK = NB * LT
    coff, CW = const_offsets()
    nc = bass.Bass("TRN2", target_bir_lowering=False)
    stack = ExitStack()
    sc = Sched(nc, stack)

    def dram_in(name, shape, dt=F32):
        return nc.dram_tensor(name, list(shape), dt, kind="ExternalInput").ap()

    def dram(name, shape, dt=F32):
        return nc.dram_tensor(name, list(shape), dt).ap()

    def sb(name, shape, dt=F32):
        return stack.enter_context(nc.sbuf_tensor(name, list(shape), dt))

    ARF, ARB = 16128, 32768
    arena = {}
    phase_off = {"f": 0, "b": 0}

    def new_phase():
        sc.barrier()
        phase_off["f"] = 0
        phase_off["b"] = 0

    def asb(name, shape, dt=F32):
        if "f" not in arena:
            arena["f"] = sb("arenaF", [128, ARF], F32)
            arena["b"] = sb("arenaB", [128, ARB], BF16)
        key = "f" if dt == F32 else "b"
        n = 1
        for d_ in shape[1:]:
            n *= d_
        n = (n + 15) // 16 * 16
        o = phase_off[key]
        phase_off[key] = o + n
        assert phase_off[key] <= (ARF if key == "f" else ARB), (name, key, phase_off[key])
        nn = 1
        for d_ in shape[1:]:
            nn *= d_
        v = arena[key][0:shape[0], o:o + nn]
        if len(shape) == 3:
            v = v.rearrange("p (a b) -> p a b", a=shape[1])
        elif len(shape) == 4:
            v = v.rearrange("p (a b c) -> p a b c", a=shape[1], b=shape[2])
        return v

    def ps(name, shape, dt=F32):
        return stack.enter_context(nc.psum_tensor(name, list(shape), dt))

    xin = dram_in("xin", [NTOK, D])
    cT_in = dram_in("cT", [128, 8, NB + 1])
    consts_in = dram_in("consts", [128, CW])
    onehot_in = dram_in("onehot", [128, 8])
    SH = NCORES
    w_ada_s = dram_in("w_ada", [DEPTH, D // SH, 6 * D])
    w_in_s = dram_in("w_in", [DEPTH, D // SH, IN_COLS])
    w_br_s = dram_in("w_br", [DEPTH, 1024 // SH, D])
    w_out_s = dram_in("w_out", [DEPTH, D // SH, D])
    w_gu_s = dram_in("w_gu", [DEPTH, E * D // SH, 2 * D])
    w_dn_s = dram_in("w_dn", [DEPTH, E * 128 // SH, 8 * D])
    w_router = dram_in("w_router", [DEPTH, 128, 8, E])
    rows_in = dram_in("rows", [DEPTH, 1, 17408])
    cols_in = dram_in("cols", [DEPTH, 128, 256])
    pool_w_in = dram_in("pool_w", [DEPTH, 64, 4, 64])
    sgu_wT_in = dram_in("sgu_wT", [DEPTH, 128, 4, 128])
    bgu_in = [dram_in("bgu%d" % l_, [E * 128, 16]) for l_ in range(DEPTH)]
    bdn_in = dram_in("bdn", [DEPTH, E, D])
    out = nc.dram_tensor("out", [NB * S, D], F32, kind="ExternalOutput").ap()

    R_BADA, R_BIN, R_LN1G, R_LN1B, R_LN2G, R_LN2B, R_NG, R_SLG, R_SLB, R_BR = 0, 6144, 11024, 12048, 13072, 14096, 15120, 15632, 15888, 16144
    C_BADA, C_BINQK, C_CW, C_CB, C_PSC, C_BS = 0, 48, 56, 80, 88, 92

    xs = dram("xs", [NTOK, D])
    wb_ada = PerLayer([dram("wb_ada_%d" % l_, [D, 6 * D], BF16) for l_ in range(DEPTH)])
    stg_d = {}
    wb_in = PerLayer([dram("wb_in_%d" % l_, [D, IN_COLS], BF16) for l_ in range(DEPTH)])
    wb_br = PerLayer([dram("wb_br_%d" % l_, [1024, D], BF16) for l_ in range(DEPTH)])
    wb_out = PerLayer([dram("wb_out_%d" % l_, [D, D], BF16) for l_ in range(DEPTH)])
    wb_gu = PerLayer([dram("wb_gu_%d" % l_, [E * D, 2 * D], BF16) for l_ in range(DEPTH)])
    wb_dn = PerLayer([dram("wb_dn_%d" % l_, [E * 128, 8 * D], BF16) for l_ in range(DEPTH)])
    zt = dram("zt", [NTOK, NTM])
    qkT = dram("qkT", [8, 128, NTOK])
    qkcd = dram("qkcd", [8, 128, NTOK], BF16)
    hb = dram("hb", [NTOK, 512])
    TS = 512
    NTILE = (4 * NTOK + TS - 1) // TS + E
    NSLOT = NTILE * TS
    Hs = dram("Hs", [NSLOT, D], BF16)
    Yd = dram("Yd", [NSLOT, D])
    h2d = dram("h2d", [NTOK, D], BF16)
    rankd = dram("rankd", [NTOK, E])
    ohd = dram("ohd", [NTOK, 4 * E])
    cwd = dram("cwd", [NTOK, E])
    cwTd = dram("cwTd", [E, NTOK])

    NCF = 13 * 128
    cf = sb("cf", [128, NCF])
    cb = sb("cb", [128, CW], BF16)
    dummy = sb("dummyj", [1, 16])
    sc.dma("sp", cf[:], consts_in[:, 0:NCF], writes=["cf"])

    def CF(name, a=0, n=None):
        o, w = coff[name]
        n = w - a if n is None else n
        assert o + a + n <= NCF
        return cf[:, o + a:o + a + n]

    def CB(name, a=0, n=None):
        o, w = coff[name]
        n = w - a if n is None else n
        return cb[:, o + a:o + a + n]

    def join(reads, tag):
        sc.op("pool", MK("memset", dummy[:], 0.0), reads=list(reads) + ["dummy"], writes=["dummy", tag])

    NSTG, NSTB = 4, 8
    stg = [asb("wst%d" % i, [128, 1024]) for i in range(NSTG)]
    stb = [asb("wsb%d" % i, [128, 1024], BF16) for i in range(NSTB)]
    bcnt = [0]
    oh = asb("oh", [128, 8])
    sc.dma("sp", oh, onehot_in, writes=["oh"])
    ccnt = [0]
    for c0 in range(0, CW, 1024):
        c1 = min(CW, c0 + 1024)
        k = ccnt[0] % 2
        ccnt[0] += 1
        sc.dma("sp", stg[k][:, 0:c1 - c0], consts_in[:, c0:c1], writes=["wst%d" % k])
        sc.op("dve", MK("tensor_copy", cb[:, c0:c1], stg[k][:, 0:c1 - c0]), reads=["wst%d" % k], writes=["cb"])
    SH = NCORES
    wlist = [("ada", w_ada_s, wb_ada, D, 6 * D), ("in", w_in_s, wb_in, D, IN_COLS), ("br", w_br_s, wb_br, 1024, D),
             ("out", w_out_s, wb_out, D, D), ("gu", w_gu_s, wb_gu, E * D, 2 * D), ("dn", w_dn_s, wb_dn, E * 128, 8 * D)]
    WTAG = {}
    for l in range(DEPTH):
        for (wn, src, dst, rows, cols) in wlist:
            rs = rows // SH
            if SH > 1:
                if wn not in stg_d:
                    stg_d[wn] = dram("stg_" + wn, [rows, cols], BF16)
                tgt = stg_d[wn]
            else:
                tgt = dst[l]
            prev_dep = [("W", wn, l - 1)] if (SH > 1 and l > 0) else []
            chunk_tags = []
            for r0 in range(0, rs, 128):
                pp = min(128, rs - r0)
                for c0 in range(0, cols, 1024):
                    c1 = min(cols, c0 + 1024)
                    n = c1 - c0
                    k = ccnt[0] % NSTG
                    ccnt[0] += 1
                    sc.dma("sp", stg[k][0:pp, 0:n], src[l, r0:r0 + pp, c0:c1], writes=["wst%d" % k])
                    for r in range(SH):
                        kb_ = bcnt[0] % NSTB
                        bcnt[0] += 1
                        use_act = (bcnt[0] % 5) in (1, 3)
                        if SH == 1:
                            if use_act:
                                sc.op("act", MK("copy", stb[kb_][0:pp, 0:n], stg[k][0:pp, 0:n]), reads=["wst%d" % k], writes=["wsb%d" % kb_])
                            else:
                                sc.op("dve", MK("tensor_copy", stb[kb_][0:pp, 0:n], stg[k][0:pp, 0:n]), reads=["wst%d" % k], writes=["wsb%d" % kb_])
                        else:
                            if use_act:
                                sc.op("act", MK("activation", stb[kb_][0:pp, 0:n], stg[k][0:pp, 0:n], AF.Copy, scale=oh[0:pp, r:r + 1]),
                                      reads=["wst%d" % k, "oh"], writes=["wsb%d" % kb_])
                            else:
                                sc.op("dve", MK("tensor_scalar", stb[kb_][0:pp, 0:n], stg[k][0:pp, 0:n], oh[0:pp, r:r + 1], None, ALU.mult),
                                      reads=["wst%d" % k, "oh"], writes=["wsb%d" % kb_])
                        tg = ("Wc", wn, l, r, r0, c0)
                        chunk_tags.append(tg)
                        sc.dma("sp", tgt[r * rs + r0:r * rs + r0 + pp, c0:c1], stb[kb_][0:pp, 0:n], reads=["wsb%d" % kb_] + prev_dep, writes=[tg])
            if SH > 1:
                join(chunk_tags, ("Ws", wn, l))
                step = max(128, (32 * 1024 * 1024) // (cols * 2) // 128 * 128)
                ar_tags = []
                for r0 in range(0, rows, step):
                    r1 = min(rows, r0 + step)
                    tg = ("Wa", wn, l, r0)
                    ar_tags.append(tg)
                    sc.op("pool", MK("collective_compute", "AllReduce", ALU.add, replica_groups=[list(range(NCORES))],
                                                                                           ins=[tgt[r0:r1, :].opt()], outs=[dst[l, r0:r1, :].opt()]),
                          reads=[("Ws", wn, l)], writes=[tg])
                join(ar_tags, ("W", wn, l))
            else:
                join(chunk_tags, ("W", wn, l))

    cols_sb = sb("cols_sb", [128, 256])
    lnbc = sb("lnbc", [128, 4, D])
    smallbc = sb("smallbc", [128, 1024])
    gbc = sb("gbc", [128, 2 * (NB + 1), D])
    modc = sb("modc", [128, 32, NB + 1])
    scT = sb("scT", [128, 8, NB + 1])
    scTb = sb("scTb", [128, 8, NB + 1], BF16)
    screp = sb("screp", [128, 8, NB + 1, 128], BF16)
    brow = sb("brow", [1, 32])
    carry = sb("carry", [128, E])
    U32 = mybir.dt.uint32
    idxg_u = sb("idxg_u", [128, NTILE * 8], U32)
    idxd_u = sb("idxd_u", [128, NTILE], U32)
    pos4_u = [sb("pos4u%d" % i, [128, 4], U32) for i in range(2)]
    NTT = NTOK // 128
    pos4_all = sb("pos4all", [128, NTT * 4], U32)
    cw4_all = sb("cw4all", [128, NTT * 4])
    xt = [sb("xt%d" % i, [128, D]) for i in range(2)]
    stats = sb("stats", [128, 2, 6])
    mv = sb("mv", [128, 2])
    rstd = sb("rstd", [128, 1])
    st2 = sb("st2", [128, 2, 6])
    mv2 = sb("mv2", [128, 2])
    rs2 = sb("rs2", [128, 1])

    psA = [ps("psA%d" % i, [128, 512]) for i in range(2)]
    psT = [ps("psT0", [128, 1024], BF16)]
    psS_ = ps("psS", [128, 512])
    psS = [psS_[:, 0:256], psS_[:, 256:512]]
    psN = ps("psN", [128, 4, 256])
    psF = ps("psF", [128, 1024])
    rr = {"a": 0, "s": 0}

    def bankA():
        rr["a"] = (rr["a"] + 1) % 2
        return rr["a"]

    def bankT():
        return 0

    def bankS():
        rr["s"] = (rr["s"] + 1) % 2
        return rr["s"]

    def mm(o, l, r, start, stop, reads, writes):
        sc.op("pe", MK("matmul", o, l, r, start=start, stop=stop), reads=reads, writes=writes, sig=stop)

    def tp(o, i, ident, reads, writes, sig=True):
        sc.op("pe", MK("transpose", o, i, ident), reads=reads, writes=writes, sig=sig)

    def layer_norm_stats(src, width, tag_r, st=stats, mv_=mv, rstd_=rstd, eps=LN_EPS, tagp=""):
        nchunk = max(1, width // 512)
        w = min(512, width)
        for j in range(nchunk):
            sc.op("dve", MK("bn_stats", st[:, j, :], src[:, j * w:(j + 1) * w]), reads=tag_r, writes=[tagp + "stats%d" % j])
        sc.op("dve", MK("bn_aggr", mv_[:], st[:, 0:nchunk, :].rearrange("p a b -> p (a b)")), reads=[tagp + "stats%d" % j for j in range(nchunk)], writes=[tagp + "mv"])
        sc.op("dve", MK("tensor_scalar", rstd_[:], mv_[:, 1:2], eps, None, ALU.add), reads=[tagp + "mv"], writes=[tagp + "rstd"])
        sc.op("act", MK("activation", rstd_[:], rstd_[:], AF.Sqrt), reads=[tagp + "rstd"], writes=[tagp + "rstd"])
        sc.op("dve", MK("reciprocal", rstd_[:], rstd_[:]), reads=[tagp + "rstd"], writes=[tagp + "rstd"])

    R_BADA, R_BIN, R_LN1G, R_LN1B, R_LN2G, R_LN2B, R_NG, R_BR = 0, 6144, 12048, 13072, 14096, 15120, 16144, 17168
    C_BADA, C_BINQK, C_CW, C_CB, C_PSC, C_BS = 0, 48, 56, 80, 88, 92

    for l in range(DEPTH):
        last = (l == DEPTH - 1)
        xsrc = xin if l == 0 else xs
        WL = lambda wn: ("W", wn, l)
        new_phase()
        rowst = asb("rowst", [1, 2048])
        rows_bf = asb("rows_bf", [1, 6144], BF16)
        wada_sb = [asb("wada%d" % i, [128, 8, 512], BF16) for i in range(2)]
        for r0_ in range(0, 6144, 2048):
            sc.dma("sp", rowst, rows_in[l, :, R_BADA + r0_:R_BADA + r0_ + 2048], writes=["rowst"])
            sc.op("pool", MK("tensor_copy", rows_bf[:, r0_:r0_ + 2048], rowst), reads=["rowst"], writes=["rows_bf"])
        sc.dma("sp", brow[:, 0:E], rows_in[l, :, R_BR:R_BR + E], writes=["brow"])
        sc.dma("sp", cols_sb[:], cols_in[l], writes=["cols_sb"])
        for i, off in enumerate([R_LN1G, R_LN1B, R_LN2G, R_LN2B]):
            sc.dma("sp", lnbc[:, i, :], rows_in[l, 0, off:off + D].partition_broadcast(128), writes=["lnbc"])
        sc.dma("sp", smallbc[:], rows_in[l, 0, R_NG:R_NG + 1024].partition_broadcast(128), writes=["smallbc"])
        if l == 0:
            sc.dma("sp", scT[:], cT_in, writes=["scT"])
            sc.op("act", MK("activation", scT[:], scT[:], AF.Silu), reads=["scT"], writes=["scT"])
            sc.op("dve", MK("tensor_copy", scTb[:], scT[:]), reads=["scT"], writes=["scTb"])
            sc.op("dve", MK("tensor_copy", screp[:], scTb[:].unsqueeze(3).to_broadcast([128, 8, NB + 1, 128])), reads=["scTb"], writes=["screp"])
        for cb_i in range(12):
            wa = wada_sb[cb_i % 2]
            wtag = "wada%d" % (cb_i % 2)
            sc.dma("sp", wa, wb_ada[l, :, cb_i * 512:(cb_i + 1) * 512].rearrange("(k p) c -> p k c", p=128), reads=[WL("ada")], writes=[wtag])
            blk = cb_i // 2
            if blk in (2, 5):
                for s_ in range(NB + 1):
                    b = bankA()
                    for k in range(8):
                        mm(psA[b][:, :], screp[:, k, s_, :], wa[:, k, :], k == 0, False, [wtag, "screp"], ["psA%d" % b])
                    mm(psA[b][:, :], CB("ones", 0, 128)[0:1, :], rows_bf[0:1, cb_i * 512:(cb_i + 1) * 512], False, True, ["cb", "rows_bf"], ["psA%d" % b])
                    gi = (0 if blk == 2 else 1) * (NB + 1) + s_
                    half = cb_i % 2
                    sc.op("act", MK("copy", gbc[:, gi, half * 512:(half + 1) * 512], psA[b][:, :]), reads=["psA%d" % b], writes=["gbc"])
            else:
                vi = {0: 0, 1: 1, 3: 2, 4: 3}[blk]
                for cc in range(4):
                    b = bankA()
                    for k in range(8):
                        mm(psA[b][:, 0:NB + 1], wa[:, k, cc * 128:(cc + 1) * 128], scTb[:, k, :], k == 0, k == 7, [wtag, "scTb"], ["psA%d" % b])
                    col = C_BADA + cb_i * 4 + cc
                    j = vi * 8 + (cb_i % 2) * 4 + cc
                    addc = 1.0 if vi in (1, 3) else 0.0
                    sc.op("dve", MK("tensor_scalar", modc[:, j, :], psA[b][:, 0:NB + 1], cols_sb[:, col:col + 1], addc, ALU.add, ALU.add),
                          reads=["psA%d" % b, "cols_sb"], writes=["modc"])

        new_phase()
        rowst = asb("rowst", [1, 2048])
        rows_bf = asb("rows_bf", [1, 6144], BF16)
        wqk_sb = asb("wqk_sb", [128, 8, 1024], BF16)
        wblk = [asb("wblk%d" % i, [128, 8, 512], BF16) for i in range(2)]
        xnb = asb("xnb", [128, D], BF16)
        GP = 4
        hT = asb("hT", [128, 8, GP * 128], BF16)
        zst = [asb("zst%d" % i, [128, 512]) for i in range(3)]
        qkst = [asb("qkst%d" % i, [128, GP * 128]) for i in range(2)]
        for r0_ in range(0, 6144, 2048):
            sc.dma("sp", rowst, rows_in[l, :, R_BIN + r0_:R_BIN + r0_ + 2048], writes=["rowst"])
            sc.op("pool", MK("tensor_copy", rows_bf[:, r0_:r0_ + 2048], rowst), reads=["rowst"], writes=["rows_bf"])
        sc.dma("sp", wqk_sb, wb_in[l, :, 256:1280].rearrange("(k p) c -> p k c", p=128), reads=[WL("in")], writes=["wqk_sb"])

        def load_x(gt):
            k = gt % 2
            sc.dma("sp", xt[k][:], xsrc[gt * 128:(gt + 1) * 128, :], reads=[("xs", gt)], writes=["xt%d" % k])

        tiles = [(b_, i) for b_ in range(NB) for i in range(NT)]
        bounds = [256, 768, 1280, 1296, 1808, NTM]
        segs = []
        c0 = 0
        while c0 < NTM:
            nb_ = min(x_ for x_ in bounds if x_ > c0)
            c1 = min(c0 + 512, nb_)
            segs.append((c0, c1))
            c0 = c1
        load_x(0)
        zcnt = [0]
        for g0_ in range(0, len(tiles), GP):
            grp = tiles[g0_:g0_ + GP]
            ng = len(grp)
            for t_, (b_, i) in enumerate(grp):
                gt = b_ * NT + i
                src_i = NB if i < 2 else b_
                if g0_ + t_ + 1 < len(tiles):
                    load_x(gt + 1)
                k = gt % 2
                layer_norm_stats(xt[k], D, ["xt%d" % k])
                sc.op("dve", MK("tensor_scalar", xnb, xt[k][:], mv[:, 0:1], rstd[:, 0:1], ALU.subtract, ALU.mult), reads=["xt%d" % k, "mv", "rstd"], writes=["xnb"])
                for kc in range(8):
                    tp(psT[0][:, kc * 128:(kc + 1) * 128], xnb[:, kc * 128:(kc + 1) * 128], CB("ident"), ["xnb", "cb"], ["psT0"], sig=(kc == 7))
                for kc in range(8):
                    sc.op("act", MK("activation", hT[:, kc, t_ * 128:(t_ + 1) * 128], psT[0][:, kc * 128:(kc + 1) * 128], AF.Identity,
                                                                     bias=modc[:, 0 + kc, src_i:src_i + 1], scale=modc[:, 8 + kc, src_i:src_i + 1]),
                          reads=["psT0", "modc"], writes=[("hT", t_)])
            gts = [b_ * NT + i for (b_, i) in grp]
            for si_, (c0, c1) in enumerate(segs):
                wc0 = c0 if c0 < 256 else c0 + 1024
                n = c1 - c0
                wk_ = si_ % 2
                sc.dma("sp", wblk[wk_][:, :, 0:n], wb_in[l, :, wc0:wc0 + n].rearrange("(k p) c -> p k c", p=128), reads=[WL("in")], writes=["wblk%d" % wk_])
                if 768 <= c0 < 1280 or c0 >= 1808:
                    f = AF.Sigmoid
                elif 1296 <= c0 < 1808:
                    f = AF.Gelu
                else:
                    f = None
                for t_, gt in enumerate(gts):
                    b = bankA()
                    for kc in range(8):
                        mm(psA[b][:, 0:n], hT[:, kc, t_ * 128:(t_ + 1) * 128], wblk[wk_][:, kc, 0:n], kc == 0, False, [("hT", t_), "wblk%d" % wk_], ["psA%d" % b])
                    mm(psA[b][:, 0:n], CB("ones", 0, 128)[0:1, :], rows_bf[0:1, wc0:wc0 + n], False, True, ["cb", "rows_bf"], ["psA%d" % b])
                    zk = zcnt[0] % 3
                    zcnt[0] += 1
                    if f is not None:
                        sc.op("act", MK("activation", zst[zk][:, 0:n], psA[b][:, 0:n], f), reads=["psA%d" % b], writes=["zst%d" % zk])
                    else:
                        sc.op("dve", MK("tensor_copy", zst[zk][:, 0:n], psA[b][:, 0:n]), reads=["psA%d" % b], writes=["zst%d" % zk])
                    sc.dma("pool", zt[gt * 128:(gt + 1) * 128, c0:c1], zst[zk][:, 0:n], reads=["zst%d" % zk], writes=[("zt", gt, si_)])
            for gt in gts:
                join([("zt", gt, si_) for si_ in range(len(segs))], ("zt", gt))
            nn = ng * 128
            gt0 = gts[0]
            for cc in range(8):
                b = bankA()
                for kc in range(8):
                    mm(psA[b][:, 0:nn], wqk_sb[:, kc, cc * 128:(cc + 1) * 128], hT[:, kc, 0:nn], kc == 0, kc == 7, [("hT", t_) for t_ in range(ng)] + ["wqk_sb"], ["psA%d" % b])
                qk_ = cc % 2
                sc.op("dve", MK("tensor_scalar", qkst[qk_][:, 0:nn], psA[b][:, 0:nn], cols_sb[:, C_BINQK + cc:C_BINQK + cc + 1], None, ALU.add),
                      reads=["psA%d" % b, "cols_sb"], writes=["qkst%d" % qk_])
                sc.dma("pool", qkT[cc, :, gt0 * 128:gt0 * 128 + nn], qkst[qk_][:, 0:nn], reads=["qkst%d" % qk_], writes=[("qkTg", gt0, cc)])
            for gt in gts:
                join([("qkTg", gt0, cc) for cc in range(8)], ("qkT", gt))

        new_phase()
        wbr_p = asb("wbr_p", [64, 4, D], BF16)
        wbr_m = asb("wbr_m", [128, 4, D], BF16)
        wbr_s = asb("wbr_s", [128, 2, D], BF16)
        wout_sb = asb("wout_sb", [128, 8, D], BF16)
        wr_sb = asb("wr_sb", [128, 8, E])
        poolw_f = asb("poolw_f", [64, 4, 64])
        poolw_b = asb("poolw_b", [64, 4, 64], BF16)
        sguw_f = asb("sguw_f", [128, 4, 128])
        sguw_b = asb("sguw_b", [128, 4, 128], BF16)
        sc.dma("sp", wbr_p, wb_br[l, 0:256, :].rearrange("(g p) c -> p g c", p=64), reads=[WL("br")], writes=["wbr_p"])
        sc.dma("sp", wbr_m, wb_br[l, 256:768, :].rearrange("(g p) c -> p g c", p=128), reads=[WL("br")], writes=["wbr_m"])
        sc.dma("sp", wbr_s, wb_br[l, 768:1024, :].rearrange("(g p) c -> p g c", p=128), reads=[WL("br")], writes=["wbr_s"])
        sc.dma("sp", wout_sb, wb_out[l].rearrange("(k p) c -> p k c", p=128), reads=[WL("out")], writes=["wout_sb"])
        sc.dma("sp", wr_sb, w_router[l], writes=["wr_sb"])
        sc.dma("sp", poolw_f, pool_w_in[l], writes=["poolw_f"])
        sc.op("dve", MK("tensor_copy", poolw_b, poolw_f), reads=["poolw_f"], writes=["poolw_b"])
        sc.dma("sp", sguw_f, sgu_wT_in[l], writes=["sguw_f"])
        sc.op("dve", MK("tensor_copy", sguw_b, sguw_f), reads=["sguw_f"], writes=["sguw_b"])

        sc.op("pool", MK("memset", carry[:], 0.0), writes=["carry"])
        CP = 1024 if S >= 1024 else S
        craw = asb("craw", [128, CP + 2])
        ctmp = asb("ctmp", [128, CP])
        cob = asb("cob", [128, CP], BF16)
        qkt = [asb("qkt%d" % i, [128, 8, 128], BF16) for i in range(2)]
        g16 = asb("g16", [128, 16])
        vt = asb("vt", [128, 512])
        vext = asb("vext", [128, 4, 132], BF16)
        vw = asb("vw", [128, 4, 132], BF16)
        e1 = asb("e1", [128, 4])
        l1 = asb("l1", [128, 4])
        gg = asb("gg", [128, 4])
        dg = asb("dg", [128, 4, 128])
        Et = asb("Et", [128, 4, 128])
        cmx = asb("cmx", [128, 4])
        Mt = asb("Mt", [128, 4])
        negM = asb("negM", [128, 4])
        bm8 = asb("bm8", [128, 8])
        mend = asb("mend", [128, 8])
        ex12 = asb("ex12", [128, 12])
        E2 = asb("E2", [128, 4, 128])
        DT = asb("DT", [128, 4, 128])
        drow = asb("drow", [128, 4, 128])
        qd = asb("qd", [128, 4, 128], BF16)
        PT = asb("PT", [128, 4, 128], BF16)
        ktok = asb("ktok", [128, 4, 128], BF16)
        rd = asb("rd", [128, 4])
        hd = asb("hd", [128, 512])
        hbt = asb("hbt", [128, 512])
        Cf = asb("Cf", [128, 4, 132])
        Cb = asb("Cb", [128, 4, 132], BF16)
        mcar = [asb("mcar0", [128, 4]), asb("mcar1", [128, 4])]
        psN2 = psN
        sc.op("pool", MK("memset", vext, 1.0), writes=["vext"])
        mstate = {"p": 0, "q": 0}
        ident3 = CF("ident").unsqueeze(1).to_broadcast([128, 4, 128])

        def mstep(b_, i, d):
            gt = b_ * NT + i
            fw = (d == 0)
            io, fo = (0, 4) if fw else (8, 12)
            tri = CF("triF") if fw else CF("triB")
            sel = CF("selF") if fw else CF("selB")
            cmm = (CF("cmF") if fw else CF("cmB")).unsqueeze(1).to_broadcast([128, 4, 128])
            dmm = (CF("dmF") if fw else CF("dmB")).unsqueeze(1).to_broadcast([128, 4, 128])
            mc = mcar[mstate["p"]]
            mcn = mcar[1 - mstate["p"]]
            mct, mcnt = "mcar%d" % mstate["p"], "mcar%d" % (1 - mstate["p"])
            mstate["p"] = 1 - mstate["p"]
            qi = mstate["q"]
            mstate["q"] = 1 - qi
            qk = qkt[qi]
            qtag = "qkt%d" % qi
            sc.dma("sp", qk, qkcd[:, :, gt * 128:(gt + 1) * 128].rearrange("k p t -> p k t"), reads=[("qkcd", b_)], writes=[qtag])
            sc.dma("sp", g16, zt[gt * 128:(gt + 1) * 128, 1280:1296], reads=[("zt", gt)], writes=["g16"])
            sc.dma("sp", vt, zt[gt * 128:(gt + 1) * 128, 256:768], reads=[("zt", gt)], writes=["vt"])
            sc.op("dve", MK("tensor_copy", vext[:, :, 0:128], vt.rearrange("p (j c) -> p j c", j=4)), reads=["vt"], writes=["vext"])
            sc.op("act", MK("activation", e1, g16[:, fo:fo + 4], AF.Exp, scale=-1.0), reads=["g16"], writes=["e1"])
            sc.op("act", MK("activation", l1, e1, AF.Ln, bias=1.0), reads=["e1"], writes=["l1"])
            bs = bankS()
            mm(psS[bs][:, 0:4], tri, l1, True, True, ["cf", "l1"], ["psS%d" % bs])
            sc.op("dve", MK("tensor_tensor", gg, g16[:, io:io + 4], psS[bs][:, 0:4], ALU.add), reads=["g16", "psS%d" % bs], writes=["gg"])
            sc.op("dve", MK("tensor_tensor", dg, ident3, gg.unsqueeze(2).to_broadcast([128, 4, 128]), ALU.mult), reads=["cf", "gg"], writes=["dg"])
            ba = bankA()
            mm(psA[ba][:, :], CF("ones"), dg.rearrange("p j c -> p (j c)"), True, True, ["cf", "dg"], ["psA%d" % ba])
            sc.op("dve", MK("tensor_tensor", Et, psA[ba][:, :].rearrange("p (j c) -> p j c", j=4), cmm, ALU.add), reads=["psA%d" % ba, "cf"], writes=["Et"])
            sc.op("dve", MK("tensor_reduce", cmx, Et, AX.X, ALU.max), reads=["Et"], writes=["cmx"])
            sc.op("dve", MK("tensor_tensor", Mt, cmx, mc, ALU.max), reads=["cmx", mct], writes=["Mt"])
            sc.op("dve", MK("tensor_tensor", bm8[:, 0:4], Mt, psS[bs][:, 0:4], ALU.subtract), reads=["Mt", "psS%d" % bs], writes=["bm8a"])
            sc.op("dve", MK("tensor_copy", bm8[:, 4:8], Mt), reads=["Mt"], writes=["bm8b"])
            bs2 = bankS()
            mm(psS[bs2][:, 0:8], sel, bm8, True, True, ["cf", "bm8a", "bm8b"], ["psS%d" % bs2])
            sc.op("dve", MK("tensor_copy", mend, psS[bs2][:, 0:8]), reads=["psS%d" % bs2], writes=["mend"])
            sc.op("dve", MK("tensor_copy", mcn, mend[:, 0:4]), reads=["mend"], writes=[mcnt])
            sc.op("dve", MK("tensor_tensor", ex12[:, 0:4], gg, mend[:, 4:8], ALU.subtract), reads=["gg", "mend"], writes=["ex12a"])
            sc.op("dve", MK("tensor_tensor", ex12[:, 4:8], mc, mend[:, 4:8], ALU.subtract), reads=[mct, "mend"], writes=["ex12b"])
            sc.op("dve", MK("tensor_tensor", ex12[:, 8:12], psS[bs][:, 0:4], Mt, ALU.subtract), reads=["psS%d" % bs, "Mt"], writes=["ex12c"])
            sc.op("act", MK("activation", ex12, ex12, AF.Exp), reads=["ex12a", "ex12b", "ex12c"], writes=["ex12a", "ex12b", "ex12c", "ex12"])
            sc.op("dve", MK("tensor_scalar", negM, Mt, -1.0, None, ALU.mult), reads=["Mt"], writes=["negM"])
            sc.op("dve", MK("tensor_tensor", dg, ident3, negM.unsqueeze(2).to_broadcast([128, 4, 128]), ALU.mult), reads=["cf", "negM"], writes=["dg"])
            ba2 = bankA()
            mm(psA[ba2][:, :], CF("ones"), dg.rearrange("p j c -> p (j c)"), True, True, ["cf", "dg"], ["psA%d" % ba2])
            sc.op("dve", MK("tensor_tensor", E2, psA[ba2][:, :].rearrange("p (j c) -> p j c", j=4), dmm, ALU.add), reads=["psA%d" % ba2, "cf"], writes=["E2"])
            for j in range(4):
                sc.op("act", MK("activation", DT[:, j, :], E2[:, j, :], AF.Exp, bias=gg[:, j:j + 1]), reads=["E2", "gg"], writes=["DT"])
                sc.op("act", MK("activation", drow[:, j, :], psA[ba2][:, j * 128:(j + 1) * 128], AF.Exp, bias=mc[:, j:j + 1]), reads=["psA%d" % ba2, mct], writes=["drow"])
            sc.op("dve", MK("tensor_tensor", qd, qk[:, 0:4, :], drow, ALU.mult), reads=[qtag, "drow"], writes=["qd"])
            ba3 = bankA()
            for j in range(4):
                mm(psA[ba3][:, j * 128:(j + 1) * 128], qk[:, 4 + j, :], qk[:, j, :], True, True, [qtag], ["psA%d" % ba3])
            sc.op("dve", MK("tensor_tensor", PT.rearrange("p j c -> p (j c)"), psA[ba3][:, :], DT.rearrange("p j c -> p (j c)"), ALU.mult),
                  reads=["psA%d" % ba3, "DT"], writes=["PT"])
            for j in range(4):
                mm(psN[:, j, 0:129], PT[:, j, :], vext[:, j, 0:129], True, False, ["PT", "vext"], ["psN"])
                mm(psN[:, j, 0:129], qd[:, j, :], Cb[:, j, 0:129], False, True, ["qd", "Cb"], ["psN"])
            sc.op("dve", MK("tensor_scalar", rd, psN[:, :, 128], -1.0, None, ALU.mult), reads=["psN"], writes=["rd"])
            sc.op("dve", MK("tensor_tensor", rd, rd, psN[:, :, 128], ALU.max), reads=["psN", "rd"], writes=["rd"])
            sc.op("dve", MK("tensor_tensor", rd, rd, ex12[:, 8:12], ALU.max), reads=["rd", "ex12"], writes=["rd"])
            sc.op("dve", MK("reciprocal", rd, rd), reads=["rd"], writes=["rd"])
            dst = hd if fw else hbt
            dtag = "hd" if fw else "hbt"
            for j in range(4):
                if j % 2:
                    sc.op("act", MK("activation", dst[:, j * 128:(j + 1) * 128], psN[:, j, 0:128], AF.Copy, scale=rd[:, j:j + 1]), reads=["psN", "rd"], writes=[dtag])
                else:
                    sc.op("dve", MK("tensor_scalar", dst[:, j * 128:(j + 1) * 128], psN[:, j, 0:128], rd[:, j:j + 1], None, ALU.mult), reads=["psN", "rd"], writes=[dtag])
            if not fw:
                sc.dma("pool", hb[gt * 128:(gt + 1) * 128, :], hbt, reads=["hbt"], writes=[("hb", gt)])
            sc.op("dve", MK("tensor_tensor", vw, vext, ex12[:, 0:4].unsqueeze(2).to_broadcast([128, 4, 132]), ALU.mult), reads=["vext", "ex12"], writes=["vw"])
            for j in range(4):
                tp(psT[0][:, j * 128:(j + 1) * 128], qk[:, 4 + j, :], CB("ident"), [qtag, "cb"], ["psT0"], sig=(j == 3))
            sc.op("act", MK("copy", ktok.rearrange("p j c -> p (j c)"), psT[0][:, 0:512]), reads=["psT0"], writes=["ktok"])
            for j in range(4):
                mm(psN2[:, j, 0:129], ktok[:, j, :], vw[:, j, 0:129], True, True, ["ktok", "vw"], ["psN"])
            for j in range(4):
                sc.op("dve", MK("scalar_tensor_tensor", Cf[:, j, 0:129], Cf[:, j, 0:129], ex12[:, 4 + j:5 + j], psN2[:, j, 0:129], ALU.mult, ALU.add),
                      reads=["Cf", "ex12", "psN"], writes=["Cf"])
            sc.op("dve", MK("tensor_copy", Cb, Cf), reads=["Cf"], writes=["Cb"])

        pt2 = asb("pt2", [128, 2, 256])
        ptb = asb("ptb", [128, 2, 256], BF16)
        pmb = asb("pmb", [64, 4, 128], BF16)
        poT = asb("poT", [64, 4, 128], BF16)
        uvt = asb("uvt", [128, 512])
        vln = asb("vln", [128, 256])
        vlb = asb("vlb", [128, 256], BF16)
        sgo = asb("sgo", [128, 256], BF16)
        ot = asb("ot", [128, 512])
        hs = asb("hs", [128, 512])
        mlo = asb("mlo", [128, 512], BF16)
        brT = asb("brT", [128, 6, 128], BF16)
        mgt = asb("mgt", [128, D])
        ymid = asb("ymid", [128, D])
        ytmp = asb("ytmp", [128, D])
        ymb = asb("ymb", [128, D], BF16)
        ymT = asb("ymT", [128, 8, 128], BF16)
        x1 = asb("x1", [128, D])
        xn2 = ytmp
        h2f = asb("h2f", [128, 8, 128])
        h2b = asb("h2b", [128, D], BF16)
        oh4 = asb("oh4", [128, 4, E])
        rk = asb("rk", [128, E])
        lg = asb("lg", [128, E])
        mx8 = asb("mx8", [128, 8])
        msk = asb("msk", [128, E])
        cwt = asb("cwt", [128, E])
        cwT = asb("cwT", [E, 128])
        ssum = asb("ssum", [128, 1])

        def fwd_rest(b_, i):
            gt = b_ * NT + i
            isctx = i < 2
            src_i = NB if isctx else b_
            rows = slice(gt * 128, (gt + 1) * 128)
            if isctx:
                g0 = b_ * NT
                sc.dma("sp", pt2[:, 0, :], zt[g0 * 128:(g0 + 1) * 128, 0:256], reads=[("zt", g0)], writes=["pt2"])
                sc.dma("sp", pt2[:, 1, :], zt[(g0 + 1) * 128:(g0 + 2) * 128, 0:256], reads=[("zt", g0 + 1)], writes=["pt2"])
            else:
                sc.dma("sp", pt2[:, 0, :], zt[rows, 0:256], reads=[("zt", gt)], writes=["pt2"])
            sc.op("dve", MK("tensor_copy", ptb, pt2), reads=["pt2"], writes=["ptb"])
            ba = bankA()
            for g in range(4):
                if isctx:
                    for j in range(2):
                        idx = g * 4 + i * 2 + j
                        mm(psA[ba][0:64, g * 128:(g + 1) * 128], ptb[:, j, g * 64:(g + 1) * 64], CB("poolC", idx * 128, 128), j == 0, j == 1, ["ptb", "cb"], ["psA%d" % ba])
                else:
                    mm(psA[ba][0:64, g * 128:(g + 1) * 128], ptb[:, 0, g * 64:(g + 1) * 64], CB("poolL", g * 128, 128), True, True, ["ptb", "cb"], ["psA%d" % ba])
            sc.op("act", MK("copy", pmb.rearrange("p g c -> p (g c)"), psA[ba][0:64, :]), reads=["psA%d" % ba], writes=["pmb"])
            ba = bankA()
            for g in range(4):
                mm(psA[ba][0:64, g * 128:(g + 1) * 128], poolw_b[:, g, :], pmb[:, g, :], True, True, ["poolw_b", "pmb"], ["psA%d" % ba])
            for g in range(4):
                sc.op("act", MK("activation", poT[:, g, :], psA[ba][0:64, g * 128:(g + 1) * 128], AF.Copy, scale=cols_sb[0:64, C_PSC + g:C_PSC + g + 1]),
                      reads=["psA%d" % ba, "cols_sb"], writes=["poT"])
            sc.dma("sp", uvt, zt[rows, 1296:1808], reads=[("zt", gt)], writes=["uvt"])
            layer_norm_stats(uvt[:, 256:512], 256, ["uvt"], st=st2, mv_=mv2, rstd_=rs2, tagp="s2")
            sc.op("dve", MK("tensor_scalar", vln, uvt[:, 256:512], mv2[:, 0:1], rs2[:, 0:1], ALU.subtract, ALU.mult), reads=["uvt", "s2mv", "s2rstd"], writes=["vln"])
            sc.op("dve", MK("tensor_tensor", vln, vln, smallbc[:, 512:768], ALU.mult), reads=["vln", "smallbc"], writes=["vln"])
            sc.op("dve", MK("tensor_tensor", vlb, vln, smallbc[:, 768:1024], ALU.add), reads=["vln", "smallbc"], writes=["vlb"])
            ba = bankA()
            for g in range(4):
                mm(psA[ba][:, g * 64:(g + 1) * 64], sguw_b[:, g, :], vlb[:, g * 64:(g + 1) * 64], True, True, ["sguw_b", "vlb"], ["psA%d" % ba])
            for g in range(4):
                sc.op("dve", MK("scalar_tensor_tensor", sgo[:, g * 64:(g + 1) * 64], psA[ba][:, g * 64:(g + 1) * 64], cols_sb[:, C_BS + g:C_BS + g + 1],
                                                                    uvt[:, g * 64:(g + 1) * 64], ALU.add, ALU.mult),
                      reads=["psA%d" % ba, "cols_sb", "uvt"], writes=["sgo"])
            sc.dma("sp", hbt, hb[rows, :], reads=[("hb", gt)], writes=["hbt"])
            sc.dma("sp", ot, zt[rows, 768:1280], reads=[("zt", gt)], writes=["ot"])
            sc.op("dve", MK("tensor_tensor", hs, hd, hbt, ALU.add), reads=["hd", "hbt"], writes=["hs"])
            for j in range(4):
                layer_norm_stats(hs[:, j * 128:(j + 1) * 128], 128, ["hs"], st=st2, mv_=mv2, rstd_=rs2, eps=HN_EPS, tagp="s2")
                sc.op("dve", MK("tensor_scalar", hs[:, j * 128:(j + 1) * 128], hs[:, j * 128:(j + 1) * 128], mv2[:, 0:1], rs2[:, 0:1], ALU.subtract, ALU.mult),
                      reads=["hs", "s2mv", "s2rstd"], writes=["hs"])
            sc.op("dve", MK("tensor_tensor", hs, hs, smallbc[:, 0:512], ALU.mult), reads=["hs", "smallbc"], writes=["hs"])
            sc.op("dve", MK("tensor_tensor", mlo, hs, ot, ALU.mult), reads=["hs", "ot"], writes=["mlo"])
            for j in range(4):
                tp(psT[0][:, j * 128:(j + 1) * 128], mlo[:, j * 128:(j + 1) * 128], CB("ident"), ["mlo", "cb"], ["psT0"], sig=False)
            for j in range(2):
                tp(psT[0][:, (4 + j) * 128:(5 + j) * 128], sgo[:, j * 128:(j + 1) * 128], CB("ident"), ["sgo", "cb"], ["psT0"], sig=(j == 1))
            sc.op("act", MK("copy", brT.rearrange("p j c -> p (j c)"), psT[0][:, 0:768]), reads=["psT0"], writes=["brT"])
            for bi, (nk, lhs_of, wsb, wtag) in enumerate([(4, lambda g: poT[:, g, :], wbr_p, "wbr_p"), (4, lambda g: brT[:, g, :], wbr_m, "wbr_m"), (2, lambda g: brT[:, 4 + g, :], wbr_s, "wbr_s")]):
                sc.dma("sp", mgt, zt[rows, 1808 + bi * D:1808 + (bi + 1) * D], reads=[("zt", gt)], writes=["mgt"])
                for half in range(2):
                    cs_ = slice(half * 512, (half + 1) * 512)
                    ba = bankA()
                    for g in range(nk):
                        mm(psA[ba][:, :], lhs_of(g), wsb[:, g, cs_], g == 0, g == nk - 1, ["poT", "brT", wtag], ["psA%d" % ba])
                    if bi == 0:
                        sc.op("dve", MK("tensor_tensor", ymid[:, cs_], psA[ba][:, :], mgt[:, cs_], ALU.mult), reads=["psA%d" % ba, "mgt"], writes=["ymid"])
                    else:
                        sc.op("dve", MK("tensor_tensor", ytmp[:, cs_], psA[ba][:, :], mgt[:, cs_], ALU.mult), reads=["psA%d" % ba, "mgt"], writes=["ytmp"])
                        if bi == 1:
                            sc.op("dve", MK("tensor_tensor", ymid[:, cs_], ymid[:, cs_], ytmp[:, cs_], ALU.add), reads=["ymid", "ytmp"], writes=["ymid"])
                        else:
                            sc.op("dve", MK("tensor_tensor", ymb[:, cs_], ymid[:, cs_], ytmp[:, cs_], ALU.add), reads=["ymid", "ytmp"], writes=["ymb"])
            for kc in range(8):
                tp(psT[0][:, kc * 128:(kc + 1) * 128], ymb[:, kc * 128:(kc + 1) * 128], CB("ident"), ["ymb", "cb"], ["psT0"], sig=(kc == 7))
            sc.op("act", MK("copy", ymT.rearrange("p j c -> p (j c)"), psT[0][:, :]), reads=["psT0"], writes=["ymT"])
            for half in range(2):
                for kc in range(8):
                    mm(psF[:, half * 512:(half + 1) * 512], ymT[:, kc, :], wout_sb[:, kc, half * 512:(half + 1) * 512], kc == 0, kc == 7, ["ymT", "wout_sb"], ["psF"])
            k = gt % 2
            sc.dma("sp", xt[k][:], xsrc[rows, :], reads=[("xs", gt)], writes=["xt%d" % k])
            sc.op("dve", MK("tensor_tensor", ytmp, psF[:, :], gbc[:, src_i, :], ALU.mult), reads=["psF", "gbc"], writes=["ytmp"])
            sc.op("dve", MK("scalar_tensor_tensor", x1, xt[k][:], ALPHA, ytmp, ALU.mult, ALU.add), reads=["xt%d" % k, "ytmp"], writes=["x1"])
            layer_norm_stats(x1, D, ["x1"])
            sc.op("dve", MK("tensor_scalar", x1, x1, mv[:, 0:1], rstd[:, 0:1], ALU.subtract, ALU.mult), reads=["x1", "mv", "rstd"], writes=["x1"])
            sc.op("dve", MK("tensor_tensor", x1, x1, lnbc[:, 0, :], ALU.mult), reads=["x1", "lnbc"], writes=["x1"])
            sc.op("dve", MK("tensor_tensor", x1, x1, lnbc[:, 1, :], ALU.add), reads=["x1", "lnbc"], writes=["x1"])
            sc.dma("pool", xs[rows, :], x1, reads=["x1"], writes=[("xs", gt)])
            layer_norm_stats(x1, D, ["x1"])
            sc.op("dve", MK("tensor_scalar", xn2, x1, mv[:, 0:1], rstd[:, 0:1], ALU.subtract, ALU.mult), reads=["x1", "mv", "rstd", "ytmp"], writes=["ytmp"])
            for half in range(2):
                ba = bankA()
                for kk in range(4):
                    kc = half * 4 + kk
                    tp(psA[ba][:, kk * 128:(kk + 1) * 128], xn2[:, kc * 128:(kc + 1) * 128], CF("ident"), ["ytmp", "cf"], ["psA%d" % ba], sig=(kk == 3))
                for kk in range(4):
                    kc = half * 4 + kk
                    sc.op("act", MK("activation", h2f[:, kc, :], psA[ba][:, kk * 128:(kk + 1) * 128], AF.Identity,
                                                                     bias=modc[:, 16 + kc, src_i:src_i + 1], scale=modc[:, 24 + kc, src_i:src_i + 1]),
                          reads=["psA%d" % ba, "modc"], writes=["h2f"])
            for half in range(2):
                ba = bankA()
                for kk in range(4):
                    kc = half * 4 + kk
                    tp(psA[ba][:, kk * 128:(kk + 1) * 128], h2f[:, kc, :], CF("ident"), ["h2f", "cf"], ["psA%d" % ba], sig=(kk == 3))
                sc.op("act" if half else "dve", (MK("copy", h2b[:, half * 512:(half + 1) * 512], psA[ba][:, :])) if half else
                      (MK("tensor_copy", h2b[:, half * 512:(half + 1) * 512], psA[ba][:, :])), reads=["psA%d" % ba], writes=["h2b"])
            sc.dma("pool", h2d[rows, :], h2b, reads=["h2b"], writes=[("h2d", gt)])
            bs = bankS()
            for kc in range(8):
                mm(psS[bs][:, 0:E], h2f[:, kc, :], wr_sb[:, kc, :], kc == 0, False, ["h2f", "wr_sb"], ["psS%d" % bs])
            mm(psS[bs][:, 0:E], CF("ones", 0, 128)[0:1, :], brow[0:1, 0:E], False, True, ["cf", "brow"], ["psS%d" % bs])
            sc.op("dve", MK("tensor_copy", lg, psS[bs][:, 0:E]), reads=["psS%d" % bs], writes=["lg"])
            sc.op("dve", MK("max", out=mx8, in_=lg), reads=["lg"], writes=["mx8"])
            sc.op("dve", MK("tensor_scalar", msk, lg, mx8[:, 3:4], None, ALU.is_ge), reads=["lg", "mx8"], writes=["msk"])
            for kq in range(4):
                sc.op("dve", MK("tensor_scalar", oh4[:, kq, :], lg, mx8[:, kq:kq + 1], None, ALU.is_ge), reads=["lg", "mx8"], writes=["oh4"])
            for kq in range(3, 0, -1):
                sc.op("dve", MK("tensor_tensor", oh4[:, kq, :], oh4[:, kq, :], oh4[:, kq - 1, :], ALU.subtract), reads=["oh4"], writes=["oh4"])
            sc.dma("pool", ohd[rows, :], oh4.rearrange("p k e -> p (k e)"), reads=["oh4"], writes=[("ohd", gt)])
            bs3 = bankS()
            mm(psS[bs3][:, 0:E], CF("triS"), msk, True, True, ["cf", "msk"], ["psS%d" % bs3])
            sc.op("dve", MK("tensor_tensor", rk, psS[bs3][:, 0:E], carry[:], ALU.add), reads=["psS%d" % bs3, "carry"], writes=["rk"])
            sc.dma("pool", rankd[rows, :], rk, reads=["rk"], writes=[("rankd", gt)])
            bs4 = bankS()
            mm(psS[bs4][:, 0:E], CF("ones"), msk, True, True, ["cf", "msk"], ["psS%d" % bs4])
            sc.op("dve", MK("tensor_tensor", carry[:], carry[:], psS[bs4][:, 0:E], ALU.add), reads=["psS%d" % bs4, "carry"], writes=["carry"])
            sc.op("dve", MK("tensor_scalar", lg, lg, mx8[:, 0:1], None, ALU.subtract), reads=["lg", "mx8"], writes=["lg"])
            sc.op("act", MK("activation", lg, lg, AF.Exp), reads=["lg"], writes=["lg"])
            sc.op("dve", MK("tensor_tensor", lg, lg, msk, ALU.mult), reads=["lg", "msk"], writes=["lg"])
            sc.op("dve", MK("tensor_reduce", ssum, lg, AX.X, ALU.add), reads=["lg"], writes=["ssum"])
            sc.op("dve", MK("reciprocal", ssum, ssum), reads=["ssum"], writes=["ssum"])
            sc.op("dve", MK("tensor_scalar", cwt, lg, ssum[:, 0:1], None, ALU.mult), reads=["lg", "ssum"], writes=["cwt"])
            sc.dma("pool", cwd[rows, :], cwt, reads=["cwt"], writes=[("cw", gt)])

        for b_ in range(NB):
            base = b_ * LT
            for cc in range(8):
                w0 = cols_sb[:, C_CW + cc:C_CW + cc + 1]
                w1 = cols_sb[:, C_CW + 8 + cc:C_CW + 8 + cc + 1]
                w2 = cols_sb[:, C_CW + 16 + cc:C_CW + 16 + cc + 1]
                bb = cols_sb[:, C_CB + cc:C_CB + cc + 1]
                for (s0_, s1_) in [(0, CTX), (CTX, LT)]:
                    for a in range(s0_, s1_, CP):
                        bnd = min(s1_, a + CP)
                        n = bnd - a
                        lo, hi = max(a - 1, s0_), min(bnd + 1, s1_)
                        off = lo - (a - 1)
                        sc.op("pool", MK("memset", craw, 0.0), writes=["craw"])
                        sc.dma("sp", craw[:, off:off + hi - lo], qkT[cc, :, base + lo:base + hi], reads=[("qkT", b_ * NT + i) for i in range(NT)], writes=["craw"])
                        sc.op("dve", MK("tensor_scalar", ctmp[:, 0:n], craw[:, 1:n + 1], w1, bb, ALU.mult, ALU.add), reads=["craw", "cols_sb"], writes=["ctmp"])
                        sc.op("dve", MK("scalar_tensor_tensor", ctmp[:, 0:n], craw[:, 0:n], w0, ctmp[:, 0:n], ALU.mult, ALU.add), reads=["craw", "ctmp", "cols_sb"], writes=["ctmp"])
                        sc.op("dve", MK("scalar_tensor_tensor", ctmp[:, 0:n], craw[:, 2:n + 2], w2, ctmp[:, 0:n], ALU.mult, ALU.add), reads=["craw", "ctmp", "cols_sb"], writes=["ctmp"])
                        if cc < 4:
                            sc.op("act", MK("activation", cob[:, 0:n], ctmp[:, 0:n], AF.Silu), reads=["ctmp"], writes=["cob"])
                        else:
                            sc.op("act", MK("activation", ctmp[:, 0:n], ctmp[:, 0:n], AF.Silu), reads=["ctmp"], writes=["ctmp"])
                            sc.op("dve", MK("tensor_scalar", cob[:, 0:n], ctmp[:, 0:n], 128.0 ** -0.5, None, ALU.mult), reads=["ctmp"], writes=["cob"])
                        sc.dma("pool", qkcd[cc, :, base + a:base + bnd], cob[:, 0:n], reads=["cob"], writes=[("qkcd", b_)])
            for d, order in [(1, [1, 0] + list(range(NT - 1, 1, -1))), (0, list(range(NT)))]:
                sc.op("pool", MK("memset", Cf, 0.0), writes=["Cf"])
                sc.op("pool", MK("memset", Cb, 0.0), writes=["Cb"])
                sc.op("pool", MK("memset", mcar[mstate["p"]], 0.0), writes=["mcar%d" % mstate["p"]])
                for i in order:
                    mstep(b_, i, d)
                    if d == 0 and not (last and i < 2):
                        fwd_rest(b_, i)

        new_phase()
        IOA = bass.IndirectOffsetOnAxis
        MAGIC = 12582912.0
        nE = asb("nE", [128, E])
        padE = asb("padE", [128, E])
        incE = asb("incE", [128, E])
        offE = asb("offE", [128, E])
        onesE = asb("onesE", [128, E])
        cmp3 = asb("cmp3", [128, NTILE, E])
        teb = asb("teb", [128, NTILE])
        idxg_f = asb("idxg_f", [128, NTILE, 8])
        idxd_f = asb("idxd_f", [128, NTILE])
        rkt = asb("rkt", [128, E])
        oht = asb("oht", [128, 4, E])
        cwl = asb("cwl", [128, E])
        prod = asb("prod", [128, 4, E])
        pos4f = asb("pos4f", [128, 4])
        gb_ = [asb("gb%d" % i, [128, TS]) for i in range(2)]
        sb_ = [asb("sgm%d" % i, [128, TS]) for i in range(2)]
        ub_ = [asb("ub%d" % i, [128, TS]) for i in range(2)]
        bguj = [asb("bguj%d" % i, [128, 16]) for i in range(2)]
        ysb = [asb("ysb%d" % i, [128, D]) for i in range(2)]
        yk = [asb("yk%d" % i, [128, D]) for i in range(2)]
        accm = asb("accm", [128, D])
        x1 = asb("x1m", [128, D])
        bdn_sb = asb("bdn_sb", [E, D])
        cwTt = asb("cwTt", [E, 128])
        hsl = asb("hsl", [128, 4, D], BF16)
        hgT = asb("hgT", [128, 8, TS], BF16)
        wgj = [asb("wgj%d" % i, [128, 8, 256], BF16) for i in range(2)]
        wd = [asb("wd%d" % i, [128, 8, D], BF16) for i in range(2)]
        actb = asb("actb", [128, 8, TS], BF16)
        sc.dma("sp", bdn_sb, bdn_in[l], writes=["bdn_sb"])
        sc.op("pool", MK("memset", onesE, 1.0), writes=["onesE"])
        sc.op("dve", MK("tensor_scalar", nE, carry[:], float(TS - 1), 1.0 / TS, ALU.add, ALU.mult), reads=["carry"], writes=["nE"])
        sc.op("dve", MK("tensor_scalar", nE, nE, -0.5 + 1.0 / 1024, MAGIC, ALU.add, ALU.add), reads=["nE"], writes=["nE"])
        sc.op("dve", MK("tensor_scalar", padE, nE, -MAGIC, float(TS), ALU.add, ALU.mult), reads=["nE"], writes=["padE"])
        sc.op("dve", MK("tensor_tensor_scan", incE, onesE, padE, 0.0, ALU.mult, ALU.add), reads=["onesE", "padE"], writes=["incE"])
        sc.op("dve", MK("tensor_tensor", offE, incE, padE, ALU.subtract), reads=["incE", "padE"], writes=["offE"])
        sc.op("dve", MK("tensor_tensor", cmp3, CF("jts", 0, NTILE).unsqueeze(2).to_broadcast([128, NTILE, E]), incE.unsqueeze(1).to_broadcast([128, NTILE, E]), ALU.is_ge),
              reads=["incE", "cf"], writes=["cmp3"])
        sc.op("dve", MK("tensor_reduce", teb, cmp3, AX.X, ALU.add), reads=["cmp3"], writes=["teb"])
        sc.op("dve", MK("tensor_scalar", teb, teb, float(E - 1), None, ALU.min), reads=["teb"], writes=["teb"])
        sc.op("dve", MK("tensor_scalar", idxg_f, teb.unsqueeze(2).to_broadcast([128, NTILE, 8]), 1024.0, None, ALU.mult), reads=["teb"], writes=["idxg_f"])
        sc.op("dve", MK("tensor_tensor", idxg_f, idxg_f, CF("cg", 0, 8).unsqueeze(1).to_broadcast([128, NTILE, 8]), ALU.add), reads=["idxg_f", "cf"], writes=["idxg_f"])
        sc.op("dve", MK("tensor_copy", idxg_u[:], idxg_f.rearrange("p j k -> p (j k)")), reads=["idxg_f"], writes=["idxg_u"])
        sc.op("dve", MK("tensor_scalar", idxd_f, teb, 128.0, CF("cg", 0, 1), ALU.mult, ALU.add), reads=["teb", "cf"], writes=["idxd_f"])
        sc.op("dve", MK("tensor_copy", idxd_u[:], idxd_f), reads=["idxd_f"], writes=["idxd_u"])

        def routed(gt):
            b_, i = gt // NT, gt % NT
            return not (last and i < 2)

        for gt in range(NTT):
            if not routed(gt):
                continue
            rows = slice(gt * 128, (gt + 1) * 128)
            sc.dma("sp", rkt, rankd[rows, :], reads=[("rankd", gt)], writes=["rkt"])
            sc.dma("sp", oht, ohd[rows, :].rearrange("p (k e) -> p k e", k=4), reads=[("ohd", gt)], writes=["oht"])
            sc.dma("sp", cwl, cwd[rows, :], reads=[("cw", gt)], writes=["cwl"])
            sc.dma("sp", hsl[:, 0, :], h2d[rows, :], reads=[("h2d", gt)], writes=["hsl"])
            sc.op("dve", MK("tensor_tensor", rkt, rkt, offE, ALU.add), reads=["rkt", "offE"], writes=["rkt"])
            sc.op("dve", MK("tensor_tensor", prod, oht, rkt.unsqueeze(1).to_broadcast([128, 4, E]), ALU.mult), reads=["oht", "rkt"], writes=["prod"])
            sc.op("dve", MK("tensor_reduce", pos4f, prod, AX.X, ALU.add), reads=["prod"], writes=["pos4f"])
            sc.op("dve", MK("tensor_copy", pos4_all[:, gt * 4:gt * 4 + 4], pos4f), reads=["pos4f"], writes=[("pos4", gt)])
            sc.op("dve", MK("tensor_tensor", prod, oht, cwl.unsqueeze(1).to_broadcast([128, 4, E]), ALU.mult), reads=["oht", "cwl", "pos4f"], writes=["prod"])
            sc.op("dve", MK("tensor_reduce", cw4_all[:, gt * 4:gt * 4 + 4], prod, AX.X, ALU.add), reads=["prod"], writes=[("cw4", gt)])
            for kq in range(4):
                sc.dma("pool", Hs[:, :], hsl[:, 0, :], reads=["hsl", ("pos4", gt)], writes=["Hs"], method="indirect_dma_start",
                       out_offset=IOA(pos4_all[:, gt * 4 + kq:gt * 4 + kq + 1], 0), in_offset=None)
        for jt in range(NTILE):
            kb = jt % 2
            sc.dma("pool", bguj[kb], bgu_in[l], reads=["idxd_u"], writes=["bguj%d" % kb], method="indirect_dma_start",
                   out_offset=None, in_offset=IOA(idxd_u[:, jt:jt + 1], 0))
            kd = jt % 2
            sc.dma("pool", wd[kd].rearrange("p f d -> p (f d)"), wb_dn[l], reads=["idxd_u", WL("dn")], writes=["wd%d" % kd], method="indirect_dma_start",
                   out_offset=None, in_offset=IOA(idxd_u[:, jt:jt + 1], 0))
            sc.dma("sp", hsl, Hs[jt * TS:(jt + 1) * TS, :].rearrange("(a p) d -> p a d", p=128), reads=["Hs"], writes=["hsl"])
            for a in range(4):
                for kc in range(8):
                    tp(psT[0][:, kc * 128:(kc + 1) * 128], hsl[:, a, kc * 128:(kc + 1) * 128], CB("ident"), ["hsl", "cb"], ["psT0"], sig=(kc == 7))
                sc.op("act" if a % 2 else "dve", (MK("copy", hgT[:, :, a * 128:(a + 1) * 128], psT[0][:, :].rearrange("p (k t) -> p k t", k=8))) if a % 2 else
                      (MK("tensor_copy", hgT[:, :, a * 128:(a + 1) * 128], psT[0][:, :].rearrange("p (k t) -> p k t", k=8))), reads=["psT0"], writes=["hgT"])
            for j in range(8):
                kg = (jt * 8 + j) % 2
                sc.dma("pool", wgj[kg].rearrange("p k c -> p (k c)"), wb_gu[l], reads=["idxg_u", WL("gu")], writes=["wgj%d" % kg], method="indirect_dma_start",
                       out_offset=None, in_offset=IOA(idxg_u[:, jt * 8 + j:jt * 8 + j + 1], 0))
                if j % 2 == 0:
                    pg, pu, tg_, tu_ = psA[0][:, 0:TS], psA[1][:, 0:TS], "psA0", "psA1"
                else:
                    pg, pu, tg_, tu_ = psN[:, 0:2, :].rearrange("p a b -> p (a b)"), psN[:, 2:4, :].rearrange("p a b -> p (a b)"), "psNa", "psNb"
                for kc in range(8):
                    mm(pg, wgj[kg][:, kc, 0:128], hgT[:, kc, :], kc == 0, kc == 7, ["wgj%d" % kg, "hgT"], [tg_])
                for kc in range(8):
                    mm(pu, wgj[kg][:, kc, 128:256], hgT[:, kc, :], kc == 0, kc == 7, ["wgj%d" % kg, "hgT"], [tu_])
                q_ = j % 2
                sc.op("dve", MK("tensor_scalar", gb_[q_], pg, bguj[kb][:, j:j + 1], 7.0, ALU.add, ALU.min), reads=[tg_, "bguj%d" % kb], writes=["gb%d" % q_])
                sc.op("act", MK("activation", sb_[q_], gb_[q_], AF.Sigmoid, scale=1.702), reads=["gb%d" % q_], writes=["sgm%d" % q_])
                sc.op("pool", MK("tensor_tensor", gb_[q_], gb_[q_], sb_[q_], ALU.mult), reads=["gb%d" % q_, "sgm%d" % q_], writes=["gb%d" % q_])
                sc.op("dve", MK("tensor_scalar", ub_[q_], pu, bguj[kb][:, 8 + j:9 + j], 7.0, ALU.add, ALU.min), reads=[tu_, "bguj%d" % kb], writes=["ub%d" % q_])
                sc.op("act", MK("activation", ub_[q_], ub_[q_], AF.Relu, bias=7.0), reads=["ub%d" % q_], writes=["ub%d" % q_])
                sc.op("dve", MK("scalar_tensor_tensor", actb[:, j, :], ub_[q_], -6.0, gb_[q_], ALU.add, ALU.mult), reads=["ub%d" % q_, "gb%d" % q_], writes=[("actb", j)])
            for tt in range(4):
                ky = tt % 2
                for half in range(2):
                    for f in range(8):
                        mm(psF[:, half * 512:(half + 1) * 512], actb[:, f, tt * 128:(tt + 1) * 128], wd[kd][:, f, half * 512:(half + 1) * 512], f == 0, f == 7,
                           [("actb", f_) for f_ in range(8)] + ["wd%d" % kd], ["psF%d" % half])
                    if half:
                        sc.op("act", MK("copy", ysb[ky][:, 512:1024], psF[:, 512:1024]), reads=["psF1"], writes=["ysb%d" % ky])
                    else:
                        sc.op("dve", MK("tensor_copy", ysb[ky][:, 0:512], psF[:, 0:512]), reads=["psF0"], writes=["ysb%d" % ky])
                r0 = jt * TS + tt * 128
                sc.dma("sp", Yd[r0:r0 + 128, :], ysb[ky], reads=["ysb%d" % ky], writes=[("Yd", jt, tt)])
        join([("Yd", jt, tt) for jt in range(NTILE) for tt in range(4)], "YdAll")
        for gt in range(NTT):
            if not routed(gt):
                continue
            b_, i = gt // NT, gt % NT
            rows = slice(gt * 128, (gt + 1) * 128)
            src_i = NB if i < 2 else b_
            k = gt % 2
            sc.dma("sp", xt[k][:], xs[rows, :], reads=[("xs", gt)], writes=["xt%d" % k])
            sc.dma("sp", cwl, cwd[rows, :], reads=[("cw", gt)], writes=["cwl"])
            bs = bankS()
            tp(psS[bs][0:E, 0:128], cwl, CF("ident"), ["cwl", "cf"], ["psS%d" % bs])
            sc.op("act", MK("copy", cwTt, psS[bs][0:E, 0:128]), reads=["psS%d" % bs], writes=["cwTt"])
            for half in range(2):
                mm(psF[:, half * 512:(half + 1) * 512], cwTt, bdn_sb[:, half * 512:(half + 1) * 512], True, True, ["cwTt", "bdn_sb"], ["psF%d" % half])
            sc.op("act", MK("copy", accm, psF[:, :]), reads=["psF0", "psF1"], writes=["accm"])
            for kq in range(4):
                ky = kq % 2
                sc.dma("pool", yk[ky], Yd[:, :], reads=["YdAll", ("pos4", gt)], writes=["yk%d" % ky], method="indirect_dma_start",
                       out_offset=None, in_offset=IOA(pos4_all[:, gt * 4 + kq:gt * 4 + kq + 1], 0))
                sc.op("dve", MK("scalar_tensor_tensor", accm, yk[ky], cw4_all[:, gt * 4 + kq:gt * 4 + kq + 1], accm, ALU.mult, ALU.add),
                      reads=["yk%d" % ky, ("cw4", gt), "accm"], writes=["accm"])
            sc.op("dve", MK("tensor_tensor", accm, accm, gbc[:, (NB + 1) + src_i, :], ALU.mult), reads=["accm", "gbc"], writes=["accm"])
            sc.op("dve", MK("scalar_tensor_tensor", x1, xt[k][:], ALPHA, accm, ALU.mult, ALU.add), reads=["xt%d" % k, "accm"], writes=["x1m"])
            layer_norm_stats(x1, D, ["x1m"])
            sc.op("dve", MK("tensor_scalar", x1, x1, mv[:, 0:1], rstd[:, 0:1], ALU.subtract, ALU.mult), reads=["x1m", "mv", "rstd"], writes=["x1m"])
            sc.op("dve", MK("tensor_tensor", x1, x1, lnbc[:, 2, :], ALU.mult), reads=["x1m", "lnbc"], writes=["x1m"])
            sc.op("dve", MK("tensor_tensor", x1, x1, lnbc[:, 3, :], ALU.add), reads=["x1m", "lnbc"], writes=["x1m"])
            if last:
                orow = b_ * S + (i - 2) * 128
                sc.dma("pool", out[orow:orow + 128, :], x1, reads=["x1m"], writes=[("out", gt)])
            else:
                sc.dma("pool", xs[rows, :], x1, reads=["x1m"], writes=[("xs", gt)])

    evs = [sc.lastw[t] for t in list(sc.lastw) if isinstance(t, tuple) and t[0] == "out"]
    sc.final_wait("pool", evs)
    sc.emit()
    return nc, stack


def _prep_inputs(inp, cfg):
    NB, S, DEPTH, E, NCORES = cfg["NB"], cfg["S"], cfg["DEPTH"], cfg["E"], cfg["NCORES"]
    f = lambda a: np.asarray(a, dtype=np.float32)
    L = DEPTH
    consts = make_consts()
    cmat = np.ascontiguousarray(np.concatenate([consts[k] for k in CONST_ORDER], axis=1).astype(np.float32))
    rows = np.zeros((L, 1, 17408), np.float32)
    rows[:, 0, 0:6144] = f(inp["b_ada"])
    rows[:, 0, 6144:6144 + IN_COLS] = f(inp["b_in"])
    rows[:, 0, 12048:13072] = f(inp["ln1_g"])
    rows[:, 0, 13072:14096] = f(inp["ln1_b"])
    rows[:, 0, 14096:15120] = f(inp["ln2_g"])
    rows[:, 0, 15120:16144] = f(inp["ln2_b"])
    rows[:, 0, 16144:16656] = f(inp["mlstm_norm_g"])
    rows[:, 0, 16656:16912] = f(inp["sgu_ln_g"])
    rows[:, 0, 16912:17168] = f(inp["sgu_ln_b"])
    rows[:, 0, 17168:17168 + E] = f(inp["b_router"])
    cols = np.zeros((L, 128, 256), np.float32)
    cols[:, :, 0:48] = f(inp["b_ada"]).reshape(L, 48, 128).transpose(0, 2, 1)
    cols[:, :, 48:56] = f(inp["b_in"])[:, 256:1280].reshape(L, 8, 128).transpose(0, 2, 1)
    cols[:, :, 56:80] = f(inp["qk_conv_w"]).reshape(L, 3, 8, 128).transpose(0, 3, 1, 2).reshape(L, 128, 24)
    cols[:, :, 80:88] = f(inp["qk_conv_b"]).reshape(L, 8, 128).transpose(0, 2, 1)
    cols[:, 0:64, 88:92] = f(inp["pool_scale"]).reshape(L, 4, 64).transpose(0, 2, 1)
    cols[:, :, 92:96] = f(inp["sgu_b"]).transpose(0, 2, 1)
    pool_w = np.ascontiguousarray(f(inp["pool_w"]).transpose(0, 2, 1, 3))
    sgu_wT = np.ascontiguousarray(f(inp["sgu_w"]).transpose(0, 3, 1, 2))
    bgu = np.ascontiguousarray(f(inp["b_gate_up"]).reshape(L, E, 16, 128).transpose(0, 1, 3, 2).reshape(L, E * 128, 16))
    bdn = np.ascontiguousarray(f(inp["b_down"]))
    w_router = np.ascontiguousarray(f(inp["w_router"]).reshape(L, 8, 128, E).transpose(0, 2, 1, 3))
    w_br = np.concatenate([f(inp["w_br_pool"]), f(inp["w_br_mlstm"]), f(inp["w_br_sgu"])], axis=1)
    w_gu = f(inp["w_gate_up"]).reshape(L, E, 8, 128, 2, 8, 128).transpose(0, 1, 5, 3, 2, 4, 6).reshape(L, E * D, 2 * D)
    w_dn = f(inp["w_down"]).reshape(L, E, 8, 128, D).transpose(0, 1, 3, 2, 4).reshape(L, E * 128, 8 * D)
    big = {"w_ada": f(inp["w_ada"]), "w_in": f(inp["w_in"]), "w_br": w_br, "w_out": f(inp["w_out"]), "w_gu": w_gu, "w_dn": w_dn}
    x, ctx, c, c_ctx = f(inp["x"]), f(inp["ctx"]), f(inp["c"]), f(inp["c_ctx"])
    maps = []
    for r in range(NCORES):
        m = {}
        bsl = range(r * NB, (r + 1) * NB)
        m["xin"] = np.ascontiguousarray(np.concatenate([np.concatenate([ctx[b], x[b]], axis=0) for b in bsl], axis=0))
        cs = np.stack([c[b] for b in bsl] + [c_ctx], axis=1)
        m["cT"] = np.ascontiguousarray(cs.reshape(8, 128, NB + 1).transpose(1, 0, 2))
        m["consts"] = cmat
        oh = np.zeros((128, 8), np.float32)
        oh[:, r] = 1.0
        m["onehot"] = oh
        for k, w in big.items():
            rs = w.shape[1] // NCORES
            m[k] = np.ascontiguousarray(w[:, r * rs:(r + 1) * rs, :])
        m["w_router"] = w_router
        m["rows"] = rows
        m["cols"] = cols
        m["pool_w"] = pool_w
        m["sgu_wT"] = sgu_wT
        for l_ in range(L):
            m["bgu%d" % l_] = np.ascontiguousarray(bgu[l_])
        m["bdn"] = bdn
        maps.append(m)
    return maps


def run(inp, cfg):
    NB, S, NCORES = cfg["NB"], cfg["S"], cfg["NCORES"]
    nc, stack = build(cfg)
    maps = _prep_inputs(inp, cfg)
    res = run_bass_kernel_spmd(nc, maps, core_ids=list(range(NCORES)))
    outs = [np.asarray(res.results[r]["out"]).reshape(NB, S, D) for r in range(NCORES)]
    return np.concatenate(outs, axis=0).astype(np.float32)


def kernel(**inp):
    cfg = dict(NB=2, S=4096, DEPTH=4, E=32, NCORES=8)
    return run(inp, cfg)
```
